# Optimizing a Trainium2 kernel written in Bass

```python
import math
import jax, jax.numpy as jnp
from jax import lax
import numpy as np

D_MODEL = 1024
BATCH = 2
SEQ = 16384
DEPTH = 2

PLE_DIM = 256
HEAD_DIM = 64
DIFF_HEADS = 8
DIFF_QK = DIFF_HEADS * 2 * HEAD_DIM
DIFF_V = DIFF_HEADS * 2 * HEAD_DIM
SWA_HEADS = 16
SWA_KV_HEADS = 2
SWA_GROUP = SWA_HEADS // SWA_KV_HEADS
SWA_Q = SWA_HEADS * HEAD_DIM
SWA_KV = SWA_KV_HEADS * HEAD_DIM
WINDOW = 128
Q_BLOCK = 128
IN_SPLITS = (DIFF_QK, DIFF_QK, DIFF_V, SWA_Q, SWA_KV, SWA_KV, D_MODEL, D_MODEL)
IN_WIDTH = sum(IN_SPLITS)
N_EXPERTS = 32
TOP_K = 4
D_FF = D_MODEL
SWIGLU_LIMIT = 7.0
SWIGLU_ALPHA = 1.702
MOE_BLOCK = 512
LN_EPS = 1e-5
RMS_EPS = 1e-5
DN_ALPHA = (2 * DEPTH) ** 0.25
DN_BETA = (8 * DEPTH) ** -0.25

kernel_name = "hybrid_diffattn_swa_sinks_moe_deepnorm"


def layer_norm(x, g, b):
    xf = x.astype(jnp.float32)
    mu = jnp.mean(xf, axis=-1, keepdims=True)
    var = jnp.mean(jnp.square(xf - mu), axis=-1, keepdims=True)
    y = (xf - mu) * lax.rsqrt(var + LN_EPS)
    return (y * g.astype(jnp.float32) + b.astype(jnp.float32)).astype(x.dtype)


def rms_norm(x, w):
    xf = x.astype(jnp.float32)
    y = xf * lax.rsqrt(jnp.mean(jnp.square(xf), axis=-1, keepdims=True) + RMS_EPS)
    return (y * w.astype(jnp.float32)).astype(x.dtype)


def alibi_slopes(n_heads):
    h = jnp.arange(1, n_heads + 1, dtype=jnp.float32)
    return jnp.exp2(-8.0 * h / n_heads)


def split_in(proj):
    idx, acc = [], 0
    for w in IN_SPLITS[:-1]:
        acc += w
        idx.append(acc)
    return jnp.split(proj, idx, axis=-1)


def diff_attention(q, k, v, lam, subln_w, lam_init):
    B, S = q.shape[0], q.shape[1]
    nb = S // Q_BLOCK
    scale = HEAD_DIM ** -0.5
    slopes = alibi_slopes(DIFF_HEADS)
    q_blocks = jnp.moveaxis(q.reshape(B, nb, Q_BLOCK, DIFF_HEADS, 2, HEAD_DIM), 1, 0)
    key_pos = jnp.arange(S)

    def one_block(args):
        qb, n = args
        s = jnp.einsum('bqhcd,bkhcd->bhcqk', qb, k).astype(jnp.float32) * scale
        dist = (n * Q_BLOCK + jnp.arange(Q_BLOCK))[:, None] - key_pos[None, :]
        s = s - slopes[None, :, None, None, None] * jnp.abs(dist).astype(jnp.float32)
        s = jnp.where(dist >= 0, s, -jnp.inf)
        prob = jax.nn.softmax(s, axis=-1)
        a = (prob[:, :, 0] - lam * prob[:, :, 1]).astype(v.dtype)
        return jnp.einsum('bhqk,bkhe->bqhe', a, v)

    o = lax.map(one_block, (q_blocks, jnp.arange(nb)))
    o = jnp.moveaxis(o, 0, 1).reshape(B, S, DIFF_HEADS, 2 * HEAD_DIM)
    o = rms_norm(o, subln_w) * (1.0 - lam_init)
    return o.reshape(B, S, DIFF_V)


def swa_attention(q, k, v, sinks):
    B, S = q.shape[0], q.shape[1]
    nb = S // Q_BLOCK
    scale = HEAD_DIM ** -0.5

    def blocks(t):
        return t.reshape((B, nb, Q_BLOCK) + t.shape[2:])

    def with_prev(t):
        prev = jnp.concatenate([jnp.zeros_like(t[:, :1]), t[:, :-1]], axis=1)
        return jnp.concatenate([prev, t], axis=2)

    qb = blocks(q)
    kb = with_prev(blocks(k))
    vb = with_prev(blocks(v))
    s = jnp.einsum('bnqhgd,bnkhd->bnhgqk', qb, kb).astype(jnp.float32) * scale
    kj = jnp.arange(2 * Q_BLOCK)
    dist = (jnp.arange(Q_BLOCK) + Q_BLOCK)[:, None] - kj[None, :]
    key_pos = jnp.arange(nb)[:, None] * Q_BLOCK - Q_BLOCK + kj[None, :]
    valid = (dist >= 0)[None] & (dist < WINDOW)[None] & (key_pos >= 0)[:, None, :]
    slopes = alibi_slopes(SWA_HEADS).reshape(SWA_KV_HEADS, SWA_GROUP)
    s = s - slopes[:, :, None, None] * jnp.abs(dist).astype(jnp.float32)
    s = jnp.where(valid[None, :, None, None], s, -jnp.inf)
    sink = sinks.astype(jnp.float32).reshape(SWA_KV_HEADS, SWA_GROUP)[:, :, None, None]
    m = jnp.maximum(jnp.max(s, axis=-1, keepdims=True), sink)
    e = jnp.exp(s - m)
    denom = jnp.sum(e, axis=-1, keepdims=True) + jnp.exp(sink - m)
    prob = (e / denom).astype(v.dtype)
    o = jnp.einsum('bnhgqk,bnkhd->bnqhgd', prob, vb)
    return o.reshape(B, S, SWA_Q)


def moe(h, w_router, b_router, w_gu, b_gu, w_down, b_down):
    B, S, D = h.shape
    N = B * S
    hf = h.reshape(N, D)
    logits = (hf @ w_router + b_router).astype(jnp.float32)
    top_logits, top_idx = lax.top_k(logits, TOP_K)
    gates = jax.nn.softmax(top_logits, axis=-1)
    A = N * TOP_K
    n_blocks = A // MOE_BLOCK + N_EXPERTS + 1
    flat_e = top_idx.reshape(A)
    order = jnp.argsort(flat_e)
    sorted_e = flat_e[order]
    counts = jnp.bincount(flat_e, length=N_EXPERTS)
    padded = (counts + MOE_BLOCK - 1) // MOE_BLOCK * MOE_BLOCK
    padded_end = jnp.cumsum(padded)
    rank = jnp.arange(A) - (jnp.cumsum(counts) - counts)[sorted_e]
    dest = (padded_end - padded)[sorted_e] + rank
    n_slots = n_blocks * MOE_BLOCK
    slot_tok = jnp.full((n_slots,), N, jnp.int32).at[dest].set((order // TOP_K).astype(jnp.int32))
    slot_gate = jnp.zeros((n_slots,), jnp.float32).at[dest].set(gates.reshape(A)[order])
    block_e = jnp.minimum(
        jnp.searchsorted(padded_end, jnp.arange(n_blocks) * MOE_BLOCK, side='right'),
        N_EXPERTS - 1)
    hf_pad = jnp.concatenate([hf, jnp.zeros((1, D), hf.dtype)], axis=0)

    def expert_block(args):
        tok, g, e = args
        xb = hf_pad[tok]
        gu = xb @ w_gu[e] + b_gu[e]
        gate, up = jnp.split(gu, 2, axis=-1)
        gate = jnp.minimum(gate, SWIGLU_LIMIT)
        up = jnp.clip(up, -SWIGLU_LIMIT, SWIGLU_LIMIT)
        act = (up + 1.0) * (gate * jax.nn.sigmoid(SWIGLU_ALPHA * gate))
        y = act @ w_down[e] + b_down[e]
        return y * g[:, None].astype(y.dtype)

    y = lax.map(expert_block, (slot_tok.reshape(n_blocks, MOE_BLOCK),
                               slot_gate.reshape(n_blocks, MOE_BLOCK), block_e))
    out = jnp.zeros((N + 1, D), y.dtype).at[slot_tok].add(y.reshape(n_slots, D))
    return out[:N].reshape(B, S, D)


def setup_inputs(seed: int = 0) -> dict:
    key = jax.random.key(seed)
    ks = jax.random.split(key, 26)
    f32 = jnp.float32

    def nrm(k, shape, scale):
        return jax.random.normal(k, shape, f32) * scale

    col_scale = jnp.concatenate([
        jnp.ones((2 * DIFF_QK,), f32), jnp.full((DIFF_V,), DN_BETA, f32),
        jnp.ones((SWA_Q + SWA_KV,), f32), jnp.full((SWA_KV,), DN_BETA, f32),
        jnp.ones((2 * D_MODEL,), f32)])
    return {
        "x": nrm(ks[0], (BATCH, SEQ, D_MODEL), 1.0),
        "p": nrm(ks[1], (DEPTH, BATCH, SEQ, PLE_DIM), 1.0),
        "w_in": nrm(ks[2], (DEPTH, D_MODEL, IN_WIDTH), D_MODEL ** -0.5) * col_scale,
        "b_in": nrm(ks[3], (DEPTH, IN_WIDTH), 0.01),
        "lambda_q1": nrm(ks[4], (DEPTH, HEAD_DIM), 0.1),
        "lambda_k1": nrm(ks[5], (DEPTH, HEAD_DIM), 0.1),
        "lambda_q2": nrm(ks[6], (DEPTH, HEAD_DIM), 0.1),
        "lambda_k2": nrm(ks[7], (DEPTH, HEAD_DIM), 0.1),
        "subln_w": 1.0 + nrm(ks[8], (DEPTH, 2 * HEAD_DIM), 0.01),
        "sinks": nrm(ks[9], (DEPTH, SWA_HEADS), 0.5),
        "w_br_diff": nrm(ks[10], (DEPTH, DIFF_V, D_MODEL), DIFF_V ** -0.5),
        "w_br_swa": nrm(ks[11], (DEPTH, SWA_Q, D_MODEL), SWA_Q ** -0.5),
        "w_out": nrm(ks[12], (DEPTH, D_MODEL, D_MODEL), D_MODEL ** -0.5 * DN_BETA),
        "b_out": nrm(ks[13], (DEPTH, D_MODEL), 0.01),
        "ln1_g": 1.0 + nrm(ks[14], (DEPTH, D_MODEL), 0.01),
        "ln1_b": nrm(ks[15], (DEPTH, D_MODEL), 0.01),
        "w_router": nrm(ks[16], (DEPTH, D_MODEL, N_EXPERTS), D_MODEL ** -0.5),
        "b_router": nrm(ks[17], (DEPTH, N_EXPERTS), 0.01),
        "w_gate_up": nrm(ks[18], (DEPTH, N_EXPERTS, D_MODEL, 2 * D_FF), D_MODEL ** -0.5),
        "b_gate_up": nrm(ks[19], (DEPTH, N_EXPERTS, 2 * D_FF), 0.01),
        "w_down": nrm(ks[20], (DEPTH, N_EXPERTS, D_FF, D_MODEL), D_FF ** -0.5 * DN_BETA),
        "b_down": nrm(ks[21], (DEPTH, N_EXPERTS, D_MODEL), 0.01),
        "w_ple_gate": nrm(ks[22], (DEPTH, D_MODEL, D_MODEL), D_MODEL ** -0.5),
        "w_ple_proj": nrm(ks[23], (DEPTH, PLE_DIM, D_MODEL), PLE_DIM ** -0.5 * DN_BETA),
        "ln2_g": 1.0 + nrm(ks[24], (DEPTH, D_MODEL), 0.01),
        "ln2_b": nrm(ks[25], (DEPTH, D_MODEL), 0.01),
    }


def reference(x, p, w_in, b_in, lambda_q1, lambda_k1, lambda_q2, lambda_k2, subln_w, sinks,
              w_br_diff, w_br_swa, w_out, b_out, ln1_g, ln1_b, w_router, b_router,
              w_gate_up, b_gate_up, w_down, b_down, w_ple_gate, w_ple_proj, ln2_g, ln2_b):
    B, S, _ = x.shape
    for i in range(DEPTH):
        lam_init = 0.8 - 0.6 * math.exp(-0.3 * i)
        proj = x @ w_in[i] + b_in[i]
        dq, dk, dv, sq, sk, sv, ga, gb = split_in(proj)
        lam = (jnp.exp(jnp.sum(lambda_q1[i].astype(jnp.float32) * lambda_k1[i].astype(jnp.float32)))
               - jnp.exp(jnp.sum(lambda_q2[i].astype(jnp.float32) * lambda_k2[i].astype(jnp.float32)))
               + lam_init)
        o_diff = diff_attention(dq.reshape(B, S, DIFF_HEADS, 2, HEAD_DIM),
                                dk.reshape(B, S, DIFF_HEADS, 2, HEAD_DIM),
                                dv.reshape(B, S, DIFF_HEADS, 2 * HEAD_DIM),
                                lam, subln_w[i], lam_init)
        o_swa = swa_attention(sq.reshape(B, S, SWA_KV_HEADS, SWA_GROUP, HEAD_DIM),
                              sk.reshape(B, S, SWA_KV_HEADS, HEAD_DIM),
                              sv.reshape(B, S, SWA_KV_HEADS, HEAD_DIM),
                              sinks[i])
        merged = (jax.nn.sigmoid(ga) * (o_diff @ w_br_diff[i])
                  + jax.nn.sigmoid(gb) * (o_swa @ w_br_swa[i]))
        mix = merged @ w_out[i] + b_out[i]
        x = layer_norm(DN_ALPHA * x + mix, ln1_g[i], ln1_b[i])
        ffn = moe(x, w_router[i], b_router[i], w_gate_up[i], b_gate_up[i], w_down[i], b_down[i])
        ple = jax.nn.sigmoid(x @ w_ple_gate[i]) * (p[i] @ w_ple_proj[i])
        x = layer_norm(DN_ALPHA * x + ffn + ple, ln2_g[i], ln2_b[i])
    return x
```

```python
import math
from contextlib import ExitStack

import numpy as np
import ml_dtypes
import concourse.bass as bass
import concourse.mybir as mybir
from concourse.bass_utils import run_bass_kernel_spmd

F32 = mybir.dt.float32
BF16 = mybir.dt.bfloat16
AF = mybir.ActivationFunctionType
ALU = mybir.AluOpType
NPBF = ml_dtypes.bfloat16

D = 1024
BATCH = 2
SEQ = 16384
DEPTH = 2
NCORES = 8
NBLK = 32
TOK = NBLK * 128
NCH = SEQ // 128
E = 32
LN_EPS = 1e-5
RMS_EPS = 1e-5
DN_ALPHA = (2 * DEPTH) ** 0.25


class Sem:
    def __init__(self, h):
        self.h = h
        self.count = 0


class Res:
    def __init__(self, name, dsem=None):
        self.name = name
        self.w = None
        self.r = {}
        self.dsem = dsem


class Eng:
    def __init__(self, name, sem, selfsync):
        self.name = name
        self.sem = sem
        self.selfsync = selfsync
        self.waited = {}
        self.ops = []


class Prog:
    def __init__(self, nc, stack):
        self.nc = nc
        self.stack = stack
        self.allsems = []
        self.engs = {}
        for name, selfsync in (("pe", False), ("act", True), ("dve", True), ("pool", True), ("sp", False)):
            self.engs[name] = Eng(name, self.new_sem("e_" + name), selfsync)
        self.nres = 0

    def new_sem(self, name):
        s = Sem(self.stack.enter_context(self.nc.semaphore(name)))
        self.allsems.append(s)
        return s

    def res(self, name=None, dma=False):
        self.nres += 1
        name = name or ("r%d" % self.nres)
        return Res(name, self.new_sem("d_%s_%d" % (name, self.nres)) if dma else None)

    def sbuf(self, name, shape, dtype):
        return self.stack.enter_context(self.nc.sbuf_tensor(name, list(shape), dtype))

    def psum(self, name, shape, dtype):
        return self.stack.enter_context(self.nc.psum_tensor(name, list(shape), dtype))

    def emit(self, eng, fn, reads=(), writes=(), dsem=None, xreads=()):
        e = self.engs[eng]
        need = {}

        def add(s, v):
            if need.get(s, 0) < v:
                need[s] = v

        for r in reads:
            if r.w is not None:
                add(*r.w)
        for r in xreads:
            if r.w is not None:
                add(*r.w)
            for s, v in r.r.items():
                if s is not e.sem:
                    add(s, v)
        reads = list(reads) + list(xreads)
        for w in writes:
            if w.w is not None:
                add(*w.w)
            for s, v in w.r.items():
                add(s, v)
        for s, v in need.items():
            if s is e.sem and not e.selfsync:
                continue
            if e.waited.get(s, 0) >= v:
                continue
            e.waited[s] = v
            e.ops.append(("wait", s, v))
        if dsem is not None:
            dsem.count += 16
            ev = (dsem, dsem.count)
            e.ops.append(("op", fn, dsem, 16))
        else:
            e.sem.count += 1
            ev = (e.sem, e.sem.count)
            e.ops.append(("op", fn, e.sem, 1))
        for r in reads:
            if r.r.get(ev[0], 0) < ev[1]:
                r.r[ev[0]] = ev[1]
        for w in writes:
            w.w = ev
            w.r = {}
        return ev

    def dma(self, eng, out, in_, reads, writes, sem_res, **kw):
        self.emit(eng, lambda h: h.dma_start(out=out, in_=in_, **kw), reads, writes, dsem=sem_res.dsem)

    def barrier(self):
        for e in self.engs.values():
            for s in self.allsems:
                if s is e.sem:
                    continue
                if s.count > e.waited.get(s, 0):
                    e.waited[s] = s.count
                    e.ops.append(("wait", s, s.count))

    def finish(self, final_eng="sp"):
        e = self.engs[final_eng]
        for s in self.allsems:
            if s is e.sem:
                continue
            if s.count > e.waited.get(s, 0):
                e.waited[s] = s.count
                e.ops.append(("wait", s, s.count))
        nc = self.nc
        with nc.Block() as block:
            def replay(eng, h):
                for op in eng.ops:
                    if op[0] == "wait":
                        h.wait_ge(op[1].h, op[2])
                    else:
                        op[1](h).then_inc(op[2].h, op[3])

            @block.tensor
            def _(h):
                replay(self.engs["pe"], h)

            @block.scalar
            def _(h):
                replay(self.engs["act"], h)

            @block.vector
            def _(h):
                replay(self.engs["dve"], h)

            @block.gpsimd
            def _(h):
                replay(self.engs["pool"], h)

            @block.sync
            def _(h):
                replay(self.engs["sp"], h)


NQKV = 4352
A_FEAT_CHUNKS = list(range(0, 16)) + list(range(24, 33))


def build_A():
    nc = bass.Bass("TRN2", target_bir_lowering=False)
    xT = nc.dram_tensor("xT", [D, TOK], F32, kind="ExternalInput").ap()
    w_in = nc.dram_tensor("w_in", [D, 6400], F32, kind="ExternalInput").ap()
    bcol_d = nc.dram_tensor("bcol", [128, 50], F32, kind="ExternalInput").ap()
    b_in = nc.dram_tensor("b_in", [1, 6400], F32, kind="ExternalInput").ap()
    qd = nc.dram_tensor("qd", [8, 128, TOK], BF16, kind="ExternalOutput").ap()
    kd = nc.dram_tensor("kd", [8, 128, TOK], BF16, kind="ExternalOutput").ap()
    qs = nc.dram_tensor("qs", [8, 128, TOK], BF16, kind="ExternalOutput").ap()
    ks = nc.dram_tensor("ks", [128, TOK], BF16, kind="ExternalOutput").ap()
    vd = nc.dram_tensor("vd", [TOK, 1024], BF16, kind="ExternalOutput").ap()
    vs = nc.dram_tensor("vs", [TOK, 128], BF16, kind="ExternalOutput").ap()
    with ExitStack() as stack:
        P = Prog(nc, stack)
        xTb = P.sbuf("xTb", [128, 8, TOK], BF16)
        wb = P.sbuf("wb", [128, 8, NQKV], BF16)
        bcol = P.sbuf("bcol_t", [128, 50], F32)
        bq8 = P.sbuf("bq8", [128, 50], F32)
        bv = P.sbuf("bv", [128, 1152], F32)
        stage = [P.sbuf("stage%d" % i, [128, TOK], BF16) for i in range(2)]
        vstage = [P.sbuf("vstage%d" % i, [128, 1152], BF16) for i in range(2)]
        ps = P.psum("ps", [128, 8, 512], F32)

        r_x = [P.res("x%d" % k, dma=True) for k in range(8)]
        r_w = [P.res("w%d" % k, dma=True) for k in range(8)]
        r_c = P.res("consts", dma=True)
        r_bq8 = P.res("bq8")
        r_stage = [P.res("stage%d" % i, dma=True) for i in range(2)]
        r_vstage = [P.res("vstage%d" % i, dma=True) for i in range(2)]
        r_ps = [P.res("ps%d" % i) for i in range(8)]
        r_out = P.res("out")

        xTv = xT.rearrange("(kc p) t -> p kc t", p=128)
        wv = w_in.rearrange("(kc p) n -> p kc n", p=128)
        P.dma("sp", bcol[:], bcol_d, [], [r_c], r_c)
        P.dma("sp", bv[:, 0:1024], b_in[0:1, 2048:3072].to_broadcast([128, 1024]), [], [r_c], r_c)
        P.dma("sp", bv[:, 1024:1152], b_in[0:1, 4224:4352].to_broadcast([128, 128]), [], [r_c], r_c)
        for k in range(8):
            P.dma("pool", wb[:, k, :], wv[:, k, 0:NQKV], [], [r_w[k]], r_w[k])
            P.dma("pool", xTb[:, k, :], xTv[:, k, :], [], [r_x[k]], r_x[k])
        P.emit("dve", lambda h: h.tensor_scalar(out=bq8[:], in0=bcol[:], scalar1=0.125, scalar2=None, op0=ALU.mult),
               [r_c], [r_bq8])

        it = 0
        for mi, m in enumerate(A_FEAT_CHUNKS):
            st = mi % 2
            is_q = (m < 8) or (24 <= m < 32)
            for tg in range(8):
                bank = it % 4
                it += 1
                for k in range(8):
                    P.emit("pe", lambda h, k=k, m=m, tg=tg, bank=bank: h.matmul(
                        ps[:, bank, :], lhsT=wb[:, k, m * 128:(m + 1) * 128], rhs=xTb[:, k, tg * 512:(tg + 1) * 512],
                        start=(k == 0), stop=(k == 7)), [r_w[k], r_x[k]], [r_ps[bank]])
                o = stage[st][:, tg * 512:(tg + 1) * 512]
                if it % 2 == 0:
                    if is_q:
                        P.emit("act", lambda h, o=o, bank=bank, m=m: h.activation(
                            out=o, in_=ps[:, bank, :], func=AF.Identity, bias=bq8[:, m:m + 1], scale=0.125),
                            [r_ps[bank], r_bq8], [r_stage[st]])
                    else:
                        P.emit("act", lambda h, o=o, bank=bank, m=m: h.activation(
                            out=o, in_=ps[:, bank, :], func=AF.Identity, bias=bcol[:, m:m + 1], scale=1.0),
                            [r_ps[bank], r_c], [r_stage[st]])
                else:
                    P.emit("dve", lambda h, o=o, bank=bank, m=m, sc=(0.125 if is_q else 1.0): h.tensor_scalar(
                        out=o, in0=ps[:, bank, :], scalar1=bcol[:, m:m + 1], scalar2=sc, op0=ALU.add, op1=ALU.mult),
                        [r_ps[bank], r_c], [r_stage[st]])
            if m < 8:
                dst = qd[m]
            elif m < 16:
                dst = kd[m - 8]
            elif m < 32:
                dst = qs[m - 24]
            else:
                dst = ks
            P.dma("sp", dst, stage[st][:], [r_stage[st]], [r_out], r_stage[st])

        for n in range(NBLK):
            st = n % 2
            banks = [4 + (n % 2) * 3 + j for j in range(3)]
            if n % 2 == 1:
                banks = [7, 4 + 0, 4 + 1]
            banks = [4, 5, 6] if n % 2 == 0 else [7, 4, 5]
            banks = [4, 5, 6]
            for j, (c0, w) in enumerate(((2048, 512), (2560, 512), (4224, 128))):
                for k in range(8):
                    P.emit("pe", lambda h, k=k, n=n, c0=c0, w=w, bank=banks[j]: h.matmul(
                        ps[:, bank, 0:w], lhsT=xTb[:, k, n * 128:(n + 1) * 128], rhs=wb[:, k, c0:c0 + w],
                        start=(k == 0), stop=(k == 7)), [r_w[k], r_x[k]], [r_ps[banks[j]]])
            for j, (o0, w) in enumerate(((0, 512), (512, 512), (1024, 128))):
                P.emit("dve", lambda h, st=st, o0=o0, w=w, bank=banks[j]: h.tensor_tensor(
                    out=vstage[st][:, o0:o0 + w], in0=ps[:, bank, 0:w], in1=bv[:, o0:o0 + w], op=ALU.add),
                    [r_ps[banks[j]], r_c], [r_vstage[st]])
            P.dma("sp", vd[n * 128:(n + 1) * 128, :], vstage[st][:, 0:1024], [r_vstage[st]], [r_out], r_vstage[st])
            P.dma("sp", vs[n * 128:(n + 1) * 128, :], vstage[st][:, 1024:1152], [r_vstage[st]], [r_out], r_vstage[st])
        P.finish()
    return nc


AXX = mybir.AxisListType.X
GQ = 2


def diff_slopes():
    return np.array([2.0 ** (-(h + 1)) for h in range(8)], np.float64)


def swa_slopes():
    return np.array([2.0 ** (-8.0 * (h + 1) / 16.0) for h in range(16)], np.float64)


def host_consts(r, layer):
    sk = np.arange(128, dtype=np.float64)
    ab = np.zeros((128, 8, 128), np.float64)
    for h, m in enumerate(diff_slopes()):
        for dp in range(128):
            dij = dp - 3 + r
            ab[:, h, dp] = -30.0 if dij < 0 else np.maximum(m * (sk - 64.0 - 128.0 * dij), -20000.0)
    diag = (sk[:, None] <= sk[None, :]).astype(np.float32)
    m4 = np.zeros((128, 4, 2, GQ, 128), np.float32)
    for dp in range(4):
        c = 3 - dp
        t = np.ones((128, 128), np.float32) if c < r else (diag if c == r else np.zeros((128, 128), np.float32))
        m4[:, dp, :, :, :] = t[:, None, None, :]
    lam_init = 0.8 - 0.6 * math.exp(-0.3 * layer)
    lconst = np.zeros((128, 4), np.float32)
    lconst[:, 0] = lam_init
    lconst[:, 1] = 1.0 - lam_init
    lconst[:, 2] = -0.5
    return {
        "abias": ab.reshape(128, 1024).astype(np.float32),
        "mask4": m4.reshape(128, 4 * 2 * GQ * 128),
        "ident": np.eye(128, dtype=np.float32),
        "lconst": lconst,
    }


def phase_diff_attn(P, nc, T, ps, psT, r_ps, r_psT, odT, r_odT):
    G = GQ
    Kt = [P.sbuf("Kt%d" % s, [128, SEQ], BF16) for s in range(2)]
    Vt = [P.sbuf("Vt%d" % s, [128, NCH, 129], BF16) for s in range(2)]
    Qt = [P.sbuf("Qt%d" % s, [128, TOK], BF16) for s in range(2)]
    PT = [P.sbuf("PT%d" % s, [128, 2, G * 128], BF16) for s in range(3)]
    osb = [P.sbuf("osb%d" % s, [128, 2, 129], F32) for s in range(2)]
    oTh = [P.sbuf("oTh%d" % s, [128, TOK], BF16) for s in range(2)]
    abias = P.sbuf("abias_t", [128, 1024], F32)
    mask4 = P.sbuf("mask4_t", [128, 4, 2, G * 128], BF16)
    ident = P.sbuf("identb", [128, 128], BF16)
    lconst = P.sbuf("lconst_t", [128, 4], F32)
    lamv = P.sbuf("lamv", [128, 4, 64], F32)
    lamt = P.sbuf("lamt", [128, 2, 64], F32)
    lams = P.sbuf("lams", [128, 4], F32)
    negl = P.sbuf("negl", [128, 1], F32)
    wsub = P.sbuf("wsub", [128, 128], F32)
    sm = [P.sbuf("sm%d" % s, [128, 8], F32) for s in range(2)]
    otmp = [P.sbuf("otmp%d" % s, [128, 128], F32) for s in range(2)]
    ofin = [P.sbuf("ofin%d" % s, [128, 128], F32) for s in range(2)]
    ojunk = P.sbuf("ojunk", [128, 128], F32)
    obf = [P.sbuf("obf%d" % s, [128, 128], BF16) for s in range(2)]

    r_K = [P.res("K%d" % s, dma=True) for s in range(2)]
    r_V = [P.res("V%d" % s, dma=True) for s in range(2)]
    r_Q = [P.res("Q%d" % s, dma=True) for s in range(2)]
    r_PT = [P.res("PT%d" % s) for s in range(3)]
    r_osb = [P.res("osb%d" % s) for s in range(2)]
    r_oTh = [P.res("oTh%d" % s, dma=True) for s in range(2)]
    r_c = P.res("dconst", dma=True)
    r_c2 = P.res("dconst2", dma=True)
    r_lam = P.res("lam")
    r_sm = [P.res("sm%d" % s) for s in range(2)]
    r_ot = [P.res("ot%d" % s) for s in range(2)]
    r_of = [P.res("of%d" % s) for s in range(2)]
    r_oj = P.res("oj")
    r_obf = [P.res("obf%d" % s) for s in range(2)]
    r_S = [r_ps[0:2], r_ps[2:4]]
    r_O = [r_ps[4], r_ps[5]]

    P.dma("sp", abias[:], T["abias"], [], [r_c], r_c)
    P.dma("sp", lconst[:], T["lconst"], [], [r_c], r_c)
    for i, nm in enumerate(("lambda_q1", "lambda_k1", "lambda_q2", "lambda_k2")):
        P.dma("sp", lamv[:, i, :], T[nm].to_broadcast([128, 64]), [], [r_c], r_c)
    P.dma("sp", wsub[:], T["subln_w"].to_broadcast([128, 128]), [], [r_c], r_c)
    P.dma("pool", mask4[:].rearrange("p a b c -> p (a b c)"), T["mask4"], [], [r_c2], r_c2)
    P.dma("pool", ident[:], T["ident"], [], [r_c2], r_c2)
    for s in range(2):
        P.emit("dve", lambda h, s=s: h.memset(Vt[s][:, :, 128:129], 1.0), [], [r_V[s]])
    P.emit("dve", lambda h: h.tensor_tensor(out=lamt[:, 0, :], in0=lamv[:, 0, :], in1=lamv[:, 1, :], op=ALU.mult), [r_c], [r_lam])
    P.emit("dve", lambda h: h.tensor_tensor(out=lamt[:, 1, :], in0=lamv[:, 2, :], in1=lamv[:, 3, :], op=ALU.mult), [r_c], [r_lam])
    P.emit("dve", lambda h: h.tensor_reduce(out=lams[:, 0:2], in_=lamt[:], axis=AXX, op=ALU.add), [r_lam], [r_lam])
    P.emit("act", lambda h: h.activation(out=lams[:, 2:4], in_=lams[:, 0:2], func=AF.Exp), [r_lam], [r_lam])
    P.emit("dve", lambda h: h.tensor_tensor(out=lams[:, 0:1], in0=lams[:, 2:3], in1=lams[:, 3:4], op=ALU.subtract), [r_lam], [r_lam])
    P.emit("dve", lambda h: h.tensor_scalar(out=negl[:], in0=lams[:, 0:1], scalar1=lconst[:, 0:1], scalar2=-1.0,
                                            op0=ALU.add, op1=ALU.mult), [r_lam, r_c], [r_lam])
    P.emit("dve", lambda h: h.tensor_scalar(out=wsub[:], in0=wsub[:], scalar1=lconst[:, 1:2], scalar2=None, op0=ALU.mult),
           [r_c], [r_c])
    for g in range(2):
        P.emit("dve", lambda h, g=g: h.memset(ps[:, 4 + g, :], 0.0), [], [r_O[g]])

    kv = T["kd_all"]
    vv = T["vd_all"].rearrange("(j p) c -> p j c", p=128)
    cnt = {"pt": 0, "fin": 0}

    def load_head(h):
        s = h % 2
        for q4 in range(4):
            P.dma("sp", Kt[s][:, q4 * 4096:(q4 + 1) * 4096], kv[h][:, q4 * 4096:(q4 + 1) * 4096], [], [r_K[s]], r_K[s])
        for j8 in range(8):
            P.dma("sp", Vt[s][:, j8 * 16:(j8 + 1) * 16, 0:128], vv[:, j8 * 16:(j8 + 1) * 16, h * 128:(h + 1) * 128],
                  [], [r_V[s]], r_V[s])
        P.dma("sp", Qt[s][:], T["qd"][h], [], [r_Q[s]], r_Q[s])

    def finalize(h, g, n):
        hs = h % 2
        f = cnt["fin"] % 2
        cnt["fin"] += 1
        Ov = ps[:, 4 + g, :].rearrange("p (c w) -> p c w", c=2)[:, :, 0:129]
        P.emit("dve", lambda e: e.tensor_copy(out=osb[f][:], in_=Ov), [], [r_osb[f]], xreads=[r_O[g]])
        P.emit("dve", lambda e: e.memset(ps[:, 4 + g, :], 0.0), [], [r_O[g]])
        s_ = sm[f]
        P.emit("dve", lambda e: e.reciprocal(out=s_[:, 0:2], in_=osb[f][:, :, 128]), [r_osb[f]], [r_sm[f]])
        P.emit("dve", lambda e: e.tensor_tensor(out=s_[:, 2:3], in0=s_[:, 1:2], in1=negl[:], op=ALU.mult), [r_sm[f], r_lam], [r_sm[f]])
        P.emit("dve", lambda e: e.tensor_scalar(out=otmp[f][:], in0=osb[f][:, 0, 0:128], scalar1=s_[:, 0:1], scalar2=None, op0=ALU.mult),
               [r_osb[f], r_sm[f]], [r_ot[f]])
        P.emit("dve", lambda e: e.scalar_tensor_tensor(out=ofin[f][:], in0=osb[f][:, 1, 0:128], scalar=s_[:, 2:3], in1=otmp[f][:],
                                                       op0=ALU.mult, op1=ALU.add), [r_osb[f], r_sm[f], r_ot[f]], [r_of[f]])
        P.emit("dve", lambda e: e.scalar_tensor_tensor(out=ojunk[:], in0=ofin[f][:], scalar=1.0, in1=ofin[f][:],
                                                       op0=ALU.mult, op1=ALU.mult, accum_out=s_[:, 3:4]), [r_of[f]], [r_oj, r_sm[f]])
        P.emit("dve", lambda e: e.tensor_scalar(out=s_[:, 4:5], in0=s_[:, 3:4], scalar1=1.0 / 128.0, scalar2=RMS_EPS,
                                                op0=ALU.mult, op1=ALU.add), [r_sm[f]], [r_sm[f]])
        P.emit("pool", lambda e: e.tensor_tensor(out=s_[:, 5:6], in0=s_[:, 4:5], in1=lconst[:, 2:3], op=ALU.pow), [r_sm[f], r_c], [r_sm[f]])
        P.emit("dve", lambda e: e.scalar_tensor_tensor(out=obf[f][:], in0=ofin[f][:], scalar=s_[:, 5:6], in1=wsub[:],
                                                       op0=ALU.mult, op1=ALU.mult), [r_of[f], r_sm[f], r_c], [r_obf[f]])
        P.emit("pe", lambda e: e.transpose(out=psT[:, 0:128], in_=obf[f][:], identity=ident[:]), [r_obf[f], r_c2], [r_psT])
        P.emit("dve", lambda e: e.tensor_copy(out=oTh[hs][:, n * 128:(n + 1) * 128], in_=psT[:, 0:128]), [], [r_oTh[hs]], xreads=[r_psT])

    load_head(0)
    for h in range(8):
        hs = h % 2
        if h + 1 < 8:
            load_head(h + 1)
        steps = []
        for m in range(NBLK // G):
            blocks = [G * m + (G - 1 - g) for g in range(G)]
            ndp = 4 * blocks[0] + 4
            for dp in range(ndp):
                act = [(g, n, 4 * n + 3 - dp) for g, n in enumerate(blocks) if dp <= 4 * n + 3]
                steps.append((dp, act))

        def emit_S(t):
            dp, act = steps[t]
            sl = t % 2
            for (g, n, j) in act:
                for c in range(2):
                    P.emit("pe", lambda e, g=g, n=n, j=j, c=c, sl=sl, hs=hs: e.matmul(
                        ps[:, 2 * sl + c, g * 128:(g + 1) * 128],
                        lhsT=Kt[hs][c * 64:(c + 1) * 64, j * 128:(j + 1) * 128],
                        rhs=Qt[hs][c * 64:(c + 1) * 64, n * 128:(n + 1) * 128], start=True, stop=True),
                        [r_K[hs], r_Q[hs]], [r_S[sl][c]])
            na = len(act)
            pt = cnt["pt"] % 3
            cnt["pt"] += 1
            P.emit("act", lambda e, sl=sl, na=na, pt=pt, dp=dp, h=h: e.activation(
                out=PT[pt][:, :, 0:na * 128], in_=ps[:, 2 * sl:2 * sl + 2, 0:na * 128], func=AF.Exp,
                bias=abias[:, h * 128 + dp:h * 128 + dp + 1], scale=1.0),
                [r_c], [r_PT[pt]], xreads=r_S[sl])
            if dp < 4:
                P.emit("dve", lambda e, na=na, pt=pt, dp=dp: e.tensor_tensor(
                    out=PT[pt][:, :, 0:na * 128], in0=PT[pt][:, :, 0:na * 128], in1=mask4[:, dp, :, 0:na * 128], op=ALU.mult),
                    [r_c2], [r_PT[pt]])
            return pt

        def emit_PV(t, pt):
            dp, act = steps[t]
            for (g, n, j) in act:
                for c in range(2):
                    P.emit("pe", lambda e, g=g, j=j, c=c, pt=pt, hs=hs: e.matmul(
                        ps[:, 4 + g, c * 256:c * 256 + 129],
                        lhsT=PT[pt][:, c, g * 128:(g + 1) * 128], rhs=Vt[hs][:, j, :],
                        start=False, stop=False, skip_group_check=True),
                        [r_PT[pt], r_V[hs]], [r_O[g]])
                if dp == 4 * n + 3:
                    finalize(h, g, n)

        pts = {}
        for t in range(len(steps) + 1):
            if t < len(steps):
                pts[t] = emit_S(t)
            if t >= 1:
                emit_PV(t - 1, pts.pop(t - 1))
        P.dma("sp", odT[h * 128:(h + 1) * 128, :], oTh[hs][:], [r_oTh[hs]], [r_odT], r_oTh[hs])


def host_consts_swa(r):
    sk = np.arange(128, dtype=np.float64)
    sl = swa_slopes()
    sb = np.zeros((128, 16, 2), np.float64)
    for h in range(16):
        sb[:, h, 0] = sl[h] * (sk - 192.0)
        sb[:, h, 1] = sl[h] * (sk - 64.0)
    cur = (sk[:, None] <= sk[None, :]).astype(np.float32)
    prev = (sk[:, None] > sk[None, :]).astype(np.float32)
    prev0 = prev if r > 0 else np.zeros_like(prev)
    sm = np.zeros((128, 3, 2, 4, 128), np.float32)
    for i, t in enumerate((prev, cur, prev0)):
        sm[:, i] = t[:, None, None, :]
    sinkc = (sk[:, None] - 64.0) * sl[None, :]
    return {"sbias": sb.reshape(128, 32).astype(np.float32), "smask": sm.reshape(128, 3 * 1024),
            "sinkc": sinkc.astype(np.float32)}


def phase_swa(P, nc, T, ps, psT, r_ps, r_psT, osT, r_osT):
    qsT = P.sbuf("qsT", [128, 8, TOK], BF16)
    ksT = P.sbuf("ksT", [128, 2, 2, TOK], BF16)
    Vs = P.sbuf("Vs", [128, NBLK, 2, 2, 65], BF16)
    stash = P.sbuf("ostash", [128, 8, TOK], BF16)
    sbias = P.sbuf("sbias_t", [128, 32], F32)
    smask = P.sbuf("smask_t", [128, 3, 1024], BF16)
    ident = P.sbuf("identb2", [128, 128], BF16)
    sterm = P.sbuf("sterm", [128, 16], F32)
    sinkc = P.sbuf("sinkc_t", [128, 16], F32)
    PT = [P.sbuf("sPT%d" % s, [128, 2, 512], BF16) for s in range(3)]
    osb = [P.sbuf("sosb%d" % s, [128, 2, 4, 65], F32) for s in range(2)]
    lt = [P.sbuf("slt%d" % s, [128, 2, 4], F32) for s in range(2)]
    obf = [P.sbuf("sobf%d" % s, [128, 512], BF16) for s in range(2)]

    r_q = P.res("sq", dma=True)
    r_k = P.res("sk", dma=True)
    r_v = P.res("sv", dma=True)
    r_c = P.res("sc", dma=True)
    r_c2 = P.res("sc2", dma=True)
    r_st = P.res("sterm")
    r_PT = [P.res("sPT%d" % s) for s in range(3)]
    r_osb = [P.res("sosb%d" % s) for s in range(2)]
    r_lt = [P.res("slt%d" % s) for s in range(2)]
    r_obf = [P.res("sobf%d" % s) for s in range(2)]
    r_stash = P.res("stash", dma=True)
    r_S = [r_ps[0:2], r_ps[2:4]]
    r_O = r_ps[4:6]

    P.dma("sp", sbias[:], T["sbias"], [], [r_c], r_c)
    P.dma("sp", sinkc[:], T["sinkc"], [], [r_c], r_c)
    P.dma("sp", sterm[:], T["sinks"].to_broadcast([128, 16]), [], [r_c], r_c)
    P.dma("pool", smask[:].rearrange("p a b -> p (a b)"), T["smask"], [], [r_c2], r_c2)
    P.dma("pool", ident[:], T["ident"], [], [r_c2], r_c2)
    for m in range(8):
        P.dma("sp", qsT[:, m, :], T["qs"][m], [], [r_q], r_q)
    for role, nm in enumerate(("ks2_prev", "ks2_cur")):
        for g in range(2):
            P.dma("sp", ksT[:, role, g, :], T[nm][g], [], [r_k], r_k)
    P.emit("dve", lambda e: e.memset(Vs[:, :, :, :, 64:65], 1.0), [], [r_v])
    for role, nm in enumerate(("vs_prev", "vs_cur")):
        src = T[nm].rearrange("(n p) (g d) -> p n g d", p=128, g=2)
        for g in range(2):
            P.dma("sp", Vs[:, :, role, g, 0:64], src[:, :, g, :], [], [r_v], r_v)
    P.emit("dve", lambda e: e.tensor_tensor(out=sterm[:], in0=sterm[:], in1=sinkc[:], op=ALU.add), [r_c], [r_st])
    P.emit("act", lambda e: e.activation(out=sterm[:], in_=sterm[:], func=AF.Exp), [r_st], [r_st])
    for b in range(2):
        P.emit("dve", lambda e, b=b: e.memset(ps[:, 4 + b, :], 0.0), [], [r_O[b]])

    cnt = {"pt": 0, "fin": 0, "step": 0}
    pending = []

    def emit_S(n, g, role):
        sl = cnt["step"] % 2
        cnt["step"] += 1
        pt = cnt["pt"] % 3
        cnt["pt"] += 1
        for a in range(4):
            for par in range(2):
                m = g * 4 + a
                P.emit("pe", lambda e, a=a, par=par, m=m, sl=sl: e.matmul(
                    ps[:, 2 * sl + par, a * 128:(a + 1) * 128],
                    lhsT=ksT[par * 64:(par + 1) * 64, role, g, n * 128:(n + 1) * 128],
                    rhs=qsT[par * 64:(par + 1) * 64, m, n * 128:(n + 1) * 128], start=True, stop=True),
                    [r_k, r_q], [r_S[sl][par]])
        for a in range(4):
            for par in range(2):
                hh = g * 8 + 2 * a + par
                P.emit("act", lambda e, a=a, par=par, hh=hh, sl=sl, pt=pt: e.activation(
                    out=PT[pt][:, par, a * 128:(a + 1) * 128], in_=ps[:, 2 * sl + par, a * 128:(a + 1) * 128],
                    func=AF.Exp, bias=sbias[:, hh * 2 + role:hh * 2 + role + 1], scale=1.0),
                    [r_c], [r_PT[pt]], xreads=[r_S[sl][par]])
        mi = 1 if role == 1 else (2 if n == 0 else 0)
        P.emit("dve", lambda e, pt=pt, mi=mi: e.tensor_tensor(
            out=PT[pt][:].rearrange("p a b -> p (a b)"), in0=PT[pt][:].rearrange("p a b -> p (a b)"),
            in1=smask[:, mi, :], op=ALU.mult), [r_c2], [r_PT[pt]])
        return pt

    def emit_PV(n, g, role, pt):
        for a in range(4):
            for par in range(2):
                P.emit("pe", lambda e, a=a, par=par, pt=pt: e.matmul(
                    ps[:, 4 + par, a * 65:(a + 1) * 65], lhsT=PT[pt][:, par, a * 128:(a + 1) * 128],
                    rhs=Vs[:, n, role, g, :], start=False, stop=False, skip_group_check=True),
                    [r_PT[pt], r_v], [r_O[par]])
        if role == 1:
            f = cnt["fin"] % 2
            cnt["fin"] += 1
            for par in range(2):
                P.emit("dve", lambda e, par=par: e.tensor_copy(
                    out=osb[f][:, par, :, :].rearrange("p a d -> p (a d)"), in_=ps[:, 4 + par, 0:260]),
                    [], [r_osb[f]], xreads=[r_O[par]])
                P.emit("dve", lambda e, par=par: e.memset(ps[:, 4 + par, 0:260], 0.0), [], [r_O[par]])
            stv = sterm[:, g * 8:(g + 1) * 8].rearrange("p (a two) -> p two a", two=2)
            P.emit("dve", lambda e: e.tensor_tensor(out=lt[f][:], in0=osb[f][:, :, :, 64], in1=stv, op=ALU.add),
                   [r_osb[f], r_st], [r_lt[f]])
            P.emit("dve", lambda e: e.reciprocal(out=lt[f][:], in_=lt[f][:]), [r_lt[f]], [r_lt[f]])
            for a in range(4):
                for par in range(2):
                    hl = 2 * a + par
                    P.emit("dve", lambda e, a=a, par=par, hl=hl: e.tensor_scalar(
                        out=obf[f][:, hl * 64:(hl + 1) * 64], in0=osb[f][:, par, a, 0:64], scalar1=lt[f][:, par, a:a + 1],
                        scalar2=None, op0=ALU.mult), [r_osb[f], r_lt[f]], [r_obf[f]])
            for k in range(4):
                P.emit("pe", lambda e, k=k: e.transpose(out=psT[:, k * 128:(k + 1) * 128], in_=obf[f][:, k * 128:(k + 1) * 128],
                                                        identity=ident[:]), [r_obf[f], r_c2], [r_psT])
            P.emit("dve", lambda e: e.tensor_copy(
                out=stash[:, g * 4:(g + 1) * 4, n * 128:(n + 1) * 128],
                in_=psT[:, 0:512].rearrange("p (k t) -> p k t", k=4)), [], [r_stash], xreads=[r_psT])

    seq = [(n, g, role) for n in range(NBLK) for g in range(2) for role in range(2)]
    prev = None
    for item in seq:
        pt = emit_S(*item)
        if prev is not None:
            emit_PV(*prev)
        prev = item + (pt,)
    emit_PV(*prev)
    for k in range(8):
        P.dma("sp", osT[k * 128:(k + 1) * 128, :], stash[:, k, :], [r_stash], [r_osT], r_stash)


def layer_norm_tok(P, y, out, gam, bet, r_y, r_out, r_gb, tmp, lconst, r_lc):
    st, mv = tmp["st"], tmp["mv"]
    r_t = tmp["r"]
    for c in range(2):
        P.emit("dve", lambda e, c=c: e.bn_stats(out=st[:, c, :], in_=y[:, c * 512:(c + 1) * 512]), [r_y], [r_t])
    P.emit("dve", lambda e: e.bn_aggr(out=mv[:, 0:2], in_=st[:].rearrange("p a b -> p (a b)")), [r_t], [r_t])
    P.emit("dve", lambda e: e.tensor_scalar(out=mv[:, 2:3], in0=mv[:, 1:2], scalar1=LN_EPS, scalar2=None, op0=ALU.add), [r_t], [r_t])
    P.emit("pool", lambda e: e.tensor_tensor(out=mv[:, 3:4], in0=mv[:, 2:3], in1=lconst[:, 2:3], op=ALU.pow), [r_t, r_lc], [r_t])
    P.emit("dve", lambda e: e.tensor_scalar(out=y[:], in0=y[:], scalar1=mv[:, 0:1], scalar2=mv[:, 3:4],
                                            op0=ALU.subtract, op1=ALU.mult), [r_t], [r_y])
    P.emit("pool", lambda e: e.tensor_tensor(out=y[:], in0=y[:], in1=gam[:], op=ALU.mult), [r_gb], [r_y])
    P.emit("dve", lambda e: e.tensor_tensor(out=out[:], in0=y[:], in1=bet[:], op=ALU.add), [r_y, r_gb], [r_out])


def phase_post(P, nc, T, ps, psT, r_ps, r_psT, odT, osT, x1s, x1T, r_x1):
    Wg = P.sbuf("Wg", [128, 8, 2048], BF16)
    Wa = P.sbuf("Wa", [128, 8, 1024], BF16)
    Wb = P.sbuf("Wb", [128, 8, 1024], BF16)
    Wo = P.sbuf("Wo", [128, 8, 1024], BF16)
    bcol = P.sbuf("bcol3", [128, 50], F32)
    lconst = P.sbuf("lconst3", [128, 4], F32)
    ident = P.sbuf("identb3", [128, 128], BF16)
    bo = P.sbuf("bo_bc", [128, 1024], F32)
    g1 = P.sbuf("g1_bc", [128, 1024], F32)
    b1 = P.sbuf("b1_bc", [128, 1024], F32)
    xTb = [P.sbuf("xTb3_%d" % s, [128, 8, 512], BF16) for s in range(2)]
    odb = [P.sbuf("odb%d" % s, [128, 8, 512], BF16) for s in range(2)]
    osb = [P.sbuf("osb3_%d" % s, [128, 8, 512], BF16) for s in range(2)]
    sg = [P.sbuf("sg%d" % s, [128, 2, 512], F32) for s in range(2)]
    t12 = [P.sbuf("t12_%d" % s, [128, 2, 512], F32) for s in range(2)]
    mT = P.sbuf("mT", [128, 8, 512], BF16)
    xin = [P.sbuf("xin%d" % s, [128, 1024], F32) for s in range(2)]
    ysb = [P.sbuf("ysb%d" % s, [128, 1024], F32) for s in range(2)]
    x1o = [P.sbuf("x1o%d" % s, [128, 1024], F32) for s in range(2)]
    x1b = [P.sbuf("x1b%d" % s, [128, 1024], BF16) for s in range(2)]
    x1Tb = [P.sbuf("x1Tb%d" % s, [128, 8, 128], BF16) for s in range(2)]
    lnt = [{"st": P.sbuf("lnst%d" % s, [128, 2, 6], F32), "mv": P.sbuf("lnmv%d" % s, [128, 4], F32), "r": P.res("lnr%d" % s)}
           for s in range(2)]

    r_w = P.res("w3", dma=True)
    r_c = P.res("c3", dma=True)
    r_xT = [P.res("xT3_%d" % s, dma=True) for s in range(2)]
    r_od = [P.res("od3_%d" % s, dma=True) for s in range(2)]
    r_os = [P.res("os3_%d" % s, dma=True) for s in range(2)]
    r_sg = [P.res("sg%d" % s) for s in range(2)]
    r_t12 = [P.res("t12_%d" % s) for s in range(2)]
    r_mT = P.res("mT")
    r_xin = [P.res("xin%d" % s, dma=True) for s in range(2)]
    r_ysb = [P.res("ysb%d" % s) for s in range(2)]
    r_x1o = [P.res("x1o%d" % s, dma=True) for s in range(2)]
    r_x1b = [P.res("x1b%d" % s) for s in range(2)]
    r_x1Tb = [P.res("x1Tb%d" % s, dma=True) for s in range(2)]

    wv = T["w_in"].rearrange("(kc p) n -> p kc n", p=128)
    for k in range(8):
        P.dma("pool", Wg[:, k, :], wv[:, k, 4352:6400], [], [r_w], r_w)
    for Wt, nm in ((Wa, "w_br_diff"), (Wb, "w_br_swa"), (Wo, "w_out")):
        v = T[nm].rearrange("(kc p) n -> p kc n", p=128)
        for k2 in range(2):
            P.dma("pool", Wt[:, k2 * 4:(k2 + 1) * 4, :], v[:, k2 * 4:(k2 + 1) * 4, :], [], [r_w], r_w)
    P.dma("pool", ident[:], T["ident"], [], [r_w], r_w)
    P.dma("sp", bcol[:], T["bcol"], [], [r_c], r_c)
    P.dma("sp", lconst[:], T["lconst"], [], [r_c], r_c)
    P.dma("sp", bo[:], T["b_out"].to_broadcast([128, 1024]), [], [r_c], r_c)
    P.dma("sp", g1[:], T["ln1_g"].to_broadcast([128, 1024]), [], [r_c], r_c)
    P.dma("sp", b1[:], T["ln1_b"].to_broadcast([128, 1024]), [], [r_c], r_c)

    xTv = T["xT"].rearrange("(kc p) t -> p kc t", p=128)
    odv = odT.rearrange("(kc p) t -> p kc t", p=128)
    osv = osT.rearrange("(kc p) t -> p kc t", p=128)
    it = 0
    nb = 0
    for tg in range(8):
        s = tg % 2
        tsl = slice(tg * 512, (tg + 1) * 512)
        P.dma("pool", xTb[s][:], xTv[:, :, tsl], [], [r_xT[s]], r_xT[s])
        P.dma("sp", odb[s][:], odv[:, :, tsl], [], [r_od[s]], r_od[s])
        P.dma("sp", osb[s][:], osv[:, :, tsl], [], [r_os[s]], r_os[s])
        for f in range(8):
            fs = f % 2
            banks = [(it * 4 + j) % 4 for j in range(4)] if False else [0, 1, 2, 3]
            it += 1
            for j, (Wt, c0, src, rs) in enumerate(((Wg, f * 128, xTb[s], r_xT[s]), (Wg, 1024 + f * 128, xTb[s], r_xT[s]),
                                                   (Wa, f * 128, odb[s], r_od[s]), (Wb, f * 128, osb[s], r_os[s]))):
                for k in range(8):
                    P.emit("pe", lambda e, j=j, Wt=Wt, c0=c0, src=src, k=k: e.matmul(
                        ps[:, banks[j], :], lhsT=Wt[:, k, c0:c0 + 128], rhs=src[:, k, :], start=(k == 0), stop=(k == 7)),
                        [r_w, rs], [r_ps[banks[j]]])
            for j in range(2):
                P.emit("act", lambda e, j=j, fs=fs, f=f: e.activation(
                    out=sg[fs][:, j, :], in_=ps[:, banks[j], :], func=AF.Sigmoid,
                    bias=bcol[:, 34 + 8 * j + f:35 + 8 * j + f], scale=1.0), [r_c], [r_sg[fs]], xreads=[r_ps[banks[j]]])
            for j in range(2):
                P.emit("dve", lambda e, j=j, fs=fs: e.tensor_tensor(
                    out=t12[fs][:, j, :], in0=sg[fs][:, j, :], in1=ps[:, banks[2 + j], :], op=ALU.mult),
                    [r_sg[fs]], [r_t12[fs]], xreads=[r_ps[banks[2 + j]]])
            P.emit("pool", lambda e, fs=fs, f=f: e.tensor_tensor(
                out=mT[:, f, :], in0=t12[fs][:, 0, :], in1=t12[fs][:, 1, :], op=ALU.add), [r_t12[fs]], [r_mT])
        for bi in range(4):
            n = tg * 4 + bi
            bs = nb % 2
            nb += 1
            P.dma("sp", xin[bs][:], T["x"][n * 128:(n + 1) * 128, :], [], [r_xin[bs]], r_xin[bs])
            for half in range(2):
                bank = 4 + half
                for k in range(8):
                    P.emit("pe", lambda e, k=k, bi=bi, half=half, bank=bank: e.matmul(
                        ps[:, bank, :], lhsT=mT[:, k, bi * 128:(bi + 1) * 128], rhs=Wo[:, k, half * 512:(half + 1) * 512],
                        start=(k == 0), stop=(k == 7)), [r_mT, r_w], [r_ps[bank]])
                P.emit("dve", lambda e, half=half, bank=bank, bs=bs: e.tensor_tensor(
                    out=ysb[bs][:, half * 512:(half + 1) * 512], in0=ps[:, bank, :], in1=bo[:, half * 512:(half + 1) * 512],
                    op=ALU.add), [r_c], [r_ysb[bs]], xreads=[r_ps[bank]])
            P.emit("dve", lambda e, bs=bs: e.scalar_tensor_tensor(
                out=ysb[bs][:], in0=xin[bs][:], scalar=DN_ALPHA, in1=ysb[bs][:], op0=ALU.mult, op1=ALU.add),
                [r_xin[bs]], [r_ysb[bs]])
            layer_norm_tok(P, ysb[bs], x1o[bs], g1, b1, r_ysb[bs], r_x1o[bs], r_c, lnt[bs], lconst, r_c)
            P.dma("sp", x1s[n * 128:(n + 1) * 128, :], x1o[bs][:], [r_x1o[bs]], [r_x1], r_x1o[bs])
            P.emit("pool", lambda e, bs=bs: e.tensor_copy(out=x1b[bs][:], in_=x1o[bs][:]), [r_x1o[bs]], [r_x1b[bs]])
            for k in range(8):
                P.emit("pe", lambda e, k=k, bs=bs: e.transpose(out=psT[:, k * 128:(k + 1) * 128], in_=x1b[bs][:, k * 128:(k + 1) * 128],
                                                               identity=ident[:]), [r_x1b[bs], r_w], [r_psT])
            P.emit("dve", lambda e, bs=bs: e.tensor_copy(out=x1Tb[bs][:].rearrange("p k t -> p (k t)"), in_=psT[:, :]),
                   [], [r_x1Tb[bs]], xreads=[r_psT])
            P.dma("sp", x1T.rearrange("(kc p) t -> p kc t", p=128)[:, :, n * 128:(n + 1) * 128], x1Tb[bs][:],
                  [r_x1Tb[bs]], [r_x1], r_x1Tb[bs])


TG4 = 1024
NB4 = TG4 // 128


def phase_moe(P, nc, T, ps, psT, r_ps, r_psT, x1s, x1T, out_x2, r_out):
    Wgu = [P.sbuf("Wgu%d" % s, [128, 8, 2048], BF16) for s in range(2)]
    Wd = [P.sbuf("Wd%d" % s, [128, 8, 1024], BF16) for s in range(2)]
    x1Tb = P.sbuf("x1Tb4", [128, 8, TG4], BF16)
    pTb = P.sbuf("pTb4", [128, 2, TG4], BF16)
    acc = P.sbuf("acc4", [128, NB4, 1024], F32)
    actT = [P.sbuf("actT%d" % s, [128, 8, 512], BF16) for s in range(2)]
    tg_ = [P.sbuf("tg4_%d" % s, [128, 512], F32) for s in range(2)]
    ts_ = [P.sbuf("ts4_%d" % s, [128, 512], F32) for s in range(2)]
    tu_ = [P.sbuf("tu4_%d" % s, [128, 512], F32) for s in range(2)]
    Wr = P.sbuf("Wr4", [128, 8, 32], BF16)
    br = P.sbuf("br4", [128, 32], F32)
    bgu = P.sbuf("bgu4", [128, 32, 16], F32)
    bdn = P.sbuf("bdn4", [32, 1024], F32)
    lconst = P.sbuf("lconst4", [128, 4], F32)
    identf = P.sbuf("identf4", [128, 128], F32)
    g2 = P.sbuf("g2_bc", [128, 1024], F32)
    b2 = P.sbuf("b2_bc", [128, 1024], F32)
    lg = [P.sbuf("lg4_%d" % s, [128, 32], F32) for s in range(2)]
    rt = [P.sbuf("rt4_%d" % s, [128, 16], F32) for s in range(2)]
    msk = [P.sbuf("msk4_%d" % s, [128, 32], F32) for s in range(2)]
    ex = [P.sbuf("ex4_%d" % s, [128, 32], F32) for s in range(2)]
    Gt = P.sbuf("Gt4", [128, NB4, 32], F32)
    GT = [P.sbuf("GT4_%d" % s, [32, 128], F32) for s in range(2)]
    sgp = [P.sbuf("sgp4_%d" % s, [128, 512], F32) for s in range(2)]
    xin = [P.sbuf("xin4_%d" % s, [128, 1024], F32) for s in range(2)]
    lnt = [{"st": P.sbuf("lnst4_%d" % s, [128, 2, 6], F32), "mv": P.sbuf("lnmv4_%d" % s, [128, 4], F32), "r": P.res("lnr4_%d" % s)}
           for s in range(2)]

    r_W = [P.res("W4_%d" % s, dma=True) for s in range(2)]
    r_c = P.res("c4", dma=True)
    r_cw = P.res("cw4", dma=True)
    r_x1T = P.res("x1T4", dma=True)
    r_pT = P.res("pT4", dma=True)
    r_acc = [P.res("acc4_%d" % b, dma=True) for b in range(NB4)]
    r_actT = [P.res("actT%d" % s) for s in range(2)]
    r_tg = [P.res("tg4_%d" % s) for s in range(2)]
    r_ts = [P.res("ts4_%d" % s) for s in range(2)]
    r_tu = [P.res("tu4_%d" % s) for s in range(2)]
    r_lg = [P.res("lg4_%d" % s) for s in range(2)]
    r_rt = [P.res("rt4_%d" % s) for s in range(2)]
    r_G = P.res("G4")
    r_GT = [P.res("GT4_%d" % s) for s in range(2)]
    r_sgp = [P.res("sgp4_%d" % s) for s in range(2)]
    r_xin = [P.res("xin4_%d" % s, dma=True) for s in range(2)]

    P.dma("pool", Wr[:], T["w_router"].rearrange("(kc p) n -> p kc n", p=128), [], [r_cw], r_cw)
    P.dma("sp", br[:], T["b_router"].to_broadcast([128, 32]), [], [r_c], r_c)
    P.dma("sp", bgu[:].rearrange("p a b -> p (a b)"), T["bgu_col"], [], [r_c], r_c)
    P.dma("sp", bdn[:], T["b_down"], [], [r_c], r_c)
    P.dma("sp", lconst[:], T["lconst"], [], [r_c], r_c)
    P.dma("sp", identf[:], T["ident"], [], [r_c], r_c)
    P.dma("sp", g2[:], T["ln2_g"].to_broadcast([128, 1024]), [], [r_c], r_c)
    P.dma("sp", b2[:], T["ln2_b"].to_broadcast([128, 1024]), [], [r_c], r_c)
    P.emit("dve", lambda e: e.tensor_scalar(out=bgu[:, :, 8:16], in0=bgu[:, :, 8:16], scalar1=1.0, scalar2=None, op0=ALU.add),
           [r_c], [r_c])

    wguv = T["w_gate_up"].rearrange("e (kc p) n -> e p kc n", p=128)
    wdv = T["w_down"].rearrange("e (kc p) n -> e p kc n", p=128)
    x1Tv = x1T.rearrange("(kc p) t -> p kc t", p=128)
    pTv = T["pT"].rearrange("(kc p) t -> p kc t", p=128)

    def load_expert(e_, s):
        for k4 in range(4):
            P.dma("pool", Wgu[s][:, k4 * 2:(k4 + 1) * 2, :], wguv[e_][:, k4 * 2:(k4 + 1) * 2, :], [], [r_W[s]], r_W[s])
        for k2 in range(2):
            P.dma("pool", Wd[s][:, k2 * 4:(k2 + 1) * 4, :], wdv[e_][:, k2 * 4:(k2 + 1) * 4, :], [], [r_W[s]], r_W[s])

    cnt = {"w": 0, "el": 0, "y": 0, "fin": 0}
    for tq in range(TOK // TG4):
        tsl = slice(tq * TG4, (tq + 1) * TG4)
        P.dma("sp", x1Tb[:], x1Tv[:, :, tsl], [], [r_x1T], r_x1T)
        P.dma("pool", pTb[:], pTv[:, :, tsl], [], [r_pT], r_pT)
        ws0 = cnt["w"] % 2
        load_expert(0, ws0)
        for b in range(NB4):
            f = b % 2
            P.emit("pe", lambda e, b=b: [e.matmul(ps[:, 6, 0:32], lhsT=x1Tb[:, k, b * 128:(b + 1) * 128], rhs=Wr[:, k, :],
                                                  start=(k == 0), stop=(k == 7)) for k in range(8)][-1],
                   [r_x1T, r_cw], [r_ps[6]])
            P.emit("dve", lambda e, f=f: e.tensor_tensor(out=lg[f][:], in0=ps[:, 6, 0:32], in1=br[:], op=ALU.add),
                   [r_c], [r_lg[f]], xreads=[r_ps[6]])
            P.emit("dve", lambda e, f=f: e.max(out=rt[f][:, 0:8], in_=lg[f][:]), [r_lg[f]], [r_rt[f]])
            P.emit("dve", lambda e, f=f: e.tensor_scalar(out=msk[f][:], in0=lg[f][:], scalar1=rt[f][:, 3:4], scalar2=None, op0=ALU.is_ge),
                   [r_lg[f], r_rt[f]], [r_lg[f]])
            P.emit("dve", lambda e, f=f: e.tensor_scalar(out=rt[f][:, 8:9], in0=rt[f][:, 0:1], scalar1=-1.0, scalar2=None, op0=ALU.mult),
                   [r_rt[f]], [r_rt[f]])
            P.emit("act", lambda e, f=f: e.activation(out=ex[f][:], in_=lg[f][:], func=AF.Exp, bias=rt[f][:, 8:9], scale=1.0),
                   [r_lg[f], r_rt[f]], [r_lg[f]])
            P.emit("dve", lambda e, f=f: e.scalar_tensor_tensor(out=ex[f][:], in0=ex[f][:], scalar=1.0, in1=msk[f][:], op0=ALU.mult,
                                                                op1=ALU.mult, accum_out=rt[f][:, 9:10]), [r_lg[f]], [r_lg[f], r_rt[f]])
            P.emit("dve", lambda e, f=f: e.reciprocal(out=rt[f][:, 10:11], in_=rt[f][:, 9:10]), [r_rt[f]], [r_rt[f]])
            P.emit("dve", lambda e, f=f, b=b: e.tensor_scalar(out=Gt[:, b, :], in0=ex[f][:], scalar1=rt[f][:, 10:11], scalar2=None, op0=ALU.mult),
                   [r_lg[f], r_rt[f]], [r_G])
            P.emit("pe", lambda e, b=b: e.transpose(out=ps[0:32, 6, 128:256], in_=Gt[:, b, :], identity=identf[:]), [r_G, r_c], [r_ps[6]])
            P.emit("dve", lambda e, f=f: e.tensor_copy(out=GT[f][:], in_=ps[0:32, 6, 128:256]), [], [r_GT[f]], xreads=[r_ps[6]])
            for half in range(2):
                bank = 4 + half
                P.emit("pe", lambda e, f=f, half=half, bank=bank: e.matmul(
                    ps[:, bank, :], lhsT=GT[f][:], rhs=bdn[:, half * 512:(half + 1) * 512], start=True, stop=True),
                    [r_GT[f], r_c], [r_ps[bank]])
                P.emit("dve", lambda e, b=b, half=half, bank=bank: e.tensor_copy(
                    out=acc[:, b, half * 512:(half + 1) * 512], in_=ps[:, bank, :]), [], [r_acc[b]], xreads=[r_ps[bank]])
        for e_ in range(E):
            ws = cnt["w"] % 2
            cnt["w"] += 1
            if e_ + 1 < E:
                load_expert(e_ + 1, (ws + 1) % 2)
            for sgi in range(TG4 // 512):
                asl = cnt["el"] % 2
                for c in range(8):
                    es = cnt["el"] % 2
                    cnt["el"] += 1
                    bg, bu = 2 * es, 2 * es + 1
                    for bank, c0 in ((bg, c * 128), (bu, 1024 + c * 128)):
                        for k in range(8):
                            P.emit("pe", lambda e, bank=bank, c0=c0, k=k, ws=ws, sgi=sgi: e.matmul(
                                ps[:, bank, :], lhsT=Wgu[ws][:, k, c0:c0 + 128], rhs=x1Tb[:, k, sgi * 512:(sgi + 1) * 512],
                                start=(k == 0), stop=(k == 7)), [r_W[ws], r_x1T], [r_ps[bank]])
                    P.emit("act", lambda e, es=es, bg=bg, c=c, e_=e_: e.activation(
                        out=tg_[es][:], in_=ps[:, bg, :], func=AF.Identity, bias=bgu[:, e_, c:c + 1], scale=1.0),
                        [r_c], [r_tg[es]], xreads=[r_ps[bg]])
                    P.emit("pool", lambda e, es=es: e.tensor_scalar(out=tg_[es][:], in0=tg_[es][:], scalar1=7.0, scalar2=None, op0=ALU.min),
                           [], [r_tg[es]])
                    P.emit("act", lambda e, es=es: e.activation(out=ts_[es][:], in_=tg_[es][:], func=AF.Sigmoid, scale=1.702),
                           [r_tg[es]], [r_ts[es]])
                    P.emit("pool", lambda e, es=es: e.tensor_tensor(out=ts_[es][:], in0=tg_[es][:], in1=ts_[es][:], op=ALU.mult),
                           [r_tg[es]], [r_ts[es]])
                    P.emit("dve", lambda e, es=es, bu=bu, c=c, e_=e_: e.tensor_scalar(
                        out=tu_[es][:], in0=ps[:, bu, :], scalar1=bgu[:, e_, 8 + c:9 + c], scalar2=8.0, op0=ALU.add, op1=ALU.min),
                        [r_c], [r_tu[es]], xreads=[r_ps[bu]])
                    P.emit("dve", lambda e, es=es, c=c, asl=asl: e.scalar_tensor_tensor(
                        out=actT[asl][:, c, :], in0=tu_[es][:], scalar=-6.0, in1=ts_[es][:], op0=ALU.max, op1=ALU.mult),
                        [r_tu[es], r_ts[es]], [r_actT[asl]])
                for bi in range(4):
                    b = sgi * 4 + bi
                    for half in range(2):
                        bank = 4 + (cnt["y"] % 3)
                        cnt["y"] += 1
                        for c in range(8):
                            P.emit("pe", lambda e, c=c, bi=bi, half=half, bank=bank, ws=ws, asl=asl: e.matmul(
                                ps[:, bank, :], lhsT=actT[asl][:, c, bi * 128:(bi + 1) * 128],
                                rhs=Wd[ws][:, c, half * 512:(half + 1) * 512], start=(c == 0), stop=(c == 7)),
                                [r_actT[asl], r_W[ws]], [r_ps[bank]])
                        P.emit("dve", lambda e, b=b, half=half, bank=bank, e_=e_: e.scalar_tensor_tensor(
                            out=acc[:, b, half * 512:(half + 1) * 512], in0=ps[:, bank, :], scalar=Gt[:, b, e_:e_ + 1],
                            in1=acc[:, b, half * 512:(half + 1) * 512], op0=ALU.mult, op1=ALU.add),
                            [r_G], [r_acc[b]], xreads=[r_ps[bank]])
        ws = cnt["w"] % 2
        cnt["w"] += 1
        wpg = T["w_ple_gate"].rearrange("(kc p) n -> p kc n", p=128)
        wpp = T["w_ple_proj"].rearrange("(kc p) n -> p kc n", p=128)
        for k2 in range(2):
            P.dma("pool", Wgu[ws][:, k2 * 4:(k2 + 1) * 4, 0:1024], wpg[:, k2 * 4:(k2 + 1) * 4, :], [], [r_W[ws]], r_W[ws])
        P.dma("pool", Wd[ws][:, 0:2, :], wpp, [], [r_W[ws]], r_W[ws])
        for b in range(NB4):
            n = tq * NB4 + b
            f = cnt["fin"] % 2
            cnt["fin"] += 1
            P.dma("sp", xin[f][:], x1s[n * 128:(n + 1) * 128, :], [], [r_xin[f]], r_xin[f])
            for half in range(2):
                bg, bp = 2 * half, 2 * half + 1
                for k in range(8):
                    P.emit("pe", lambda e, k=k, b=b, half=half, bg=bg, ws=ws: e.matmul(
                        ps[:, bg, :], lhsT=x1Tb[:, k, b * 128:(b + 1) * 128], rhs=Wgu[ws][:, k, half * 512:(half + 1) * 512],
                        start=(k == 0), stop=(k == 7)), [r_x1T, r_W[ws]], [r_ps[bg]])
                for k in range(2):
                    P.emit("pe", lambda e, k=k, b=b, half=half, bp=bp, ws=ws: e.matmul(
                        ps[:, bp, :], lhsT=pTb[:, k, b * 128:(b + 1) * 128], rhs=Wd[ws][:, k, half * 512:(half + 1) * 512],
                        start=(k == 0), stop=(k == 1)), [r_pT, r_W[ws]], [r_ps[bp]])
                P.emit("act", lambda e, half=half, bg=bg: e.activation(out=sgp[half][:], in_=ps[:, bg, :], func=AF.Sigmoid),
                       [], [r_sgp[half]], xreads=[r_ps[bg]])
                P.emit("dve", lambda e, half=half, bp=bp: e.tensor_tensor(out=sgp[half][:], in0=sgp[half][:], in1=ps[:, bp, :], op=ALU.mult),
                       [], [r_sgp[half]], xreads=[r_ps[bp]])
                P.emit("pool", lambda e, half=half, b=b: e.tensor_tensor(
                    out=acc[:, b, half * 512:(half + 1) * 512], in0=acc[:, b, half * 512:(half + 1) * 512], in1=sgp[half][:], op=ALU.add),
                    [r_sgp[half]], [r_acc[b]])
            P.emit("dve", lambda e, b=b, f=f: e.scalar_tensor_tensor(
                out=acc[:, b, :], in0=xin[f][:], scalar=DN_ALPHA, in1=acc[:, b, :], op0=ALU.mult, op1=ALU.add),
                [r_xin[f]], [r_acc[b]])
            layer_norm_tok(P, acc[:, b, :], acc[:, b, :], g2, b2, r_acc[b], r_acc[b], r_c, lnt[f], lconst, r_c)
            P.dma("sp", out_x2[n * 128:(n + 1) * 128, :], acc[:, b, :], [r_acc[b]], [r_out], r_acc[b])


def build_B():
    nc = bass.Bass("TRN2", target_bir_lowering=False)
    T = {}

    def din(name, shape, dt=F32):
        T[name] = nc.dram_tensor(name, list(shape), dt, kind="ExternalInput").ap()

    din("qd", [8, 128, TOK], BF16)
    din("kd_all", [8, 128, SEQ], BF16)
    din("vd_all", [SEQ, 1024], BF16)
    din("abias", [128, 1024])
    din("mask4", [128, 4 * 2 * GQ * 128])
    din("ident", [128, 128])
    din("lconst", [128, 4])
    for nm in ("lambda_q1", "lambda_k1", "lambda_q2", "lambda_k2"):
        din(nm, [1, 64])
    din("subln_w", [1, 128])
    din("qs", [8, 128, TOK], BF16)
    din("ks2_prev", [2, 128, TOK], BF16)
    din("ks2_cur", [2, 128, TOK], BF16)
    din("vs_prev", [TOK, 128], BF16)
    din("vs_cur", [TOK, 128], BF16)
    din("sbias", [128, 32])
    din("smask", [128, 3 * 1024])
    din("sinkc", [128, 16])
    din("sinks", [1, 16])
    din("w_in", [D, 6400])
    din("bcol", [128, 50])
    din("w_br_diff", [D, D])
    din("w_br_swa", [D, D])
    din("w_out", [D, D])
    din("b_out", [1, D])
    din("ln1_g", [1, D])
    din("ln1_b", [1, D])
    din("xT", [D, TOK])
    din("x", [TOK, D])
    din("w_router", [D, E])
    din("b_router", [1, E])
    din("bgu_col", [128, E * 16])
    din("b_down", [E, D])
    din("w_gate_up", [E, D, 2 * D])
    din("w_down", [E, D, D])
    din("w_ple_gate", [D, D])
    din("w_ple_proj", [256, D])
    din("pT", [256, TOK])
    din("ln2_g", [1, D])
    din("ln2_b", [1, D])
    x2 = nc.dram_tensor("x2", [TOK, D], F32, kind="ExternalOutput").ap()
    odT = nc.dram_tensor("odT", [1024, TOK], BF16, kind="Internal").ap()
    osT = nc.dram_tensor("osT", [1024, TOK], BF16, kind="Internal").ap()
    x1s = nc.dram_tensor("x1s", [TOK, D], F32, kind="Internal").ap()
    x1T = nc.dram_tensor("x1T", [D, TOK], BF16, kind="Internal").ap()
    with ExitStack() as stack:
        P = Prog(nc, stack)
        ps = P.psum("ps", [128, 7, 512], F32)
        psT = P.psum("psT", [128, 1024], BF16)
        r_ps = [P.res("psb%d" % i) for i in range(7)]
        r_psT = P.res("psT")
        r_scr = P.res("scratch")
        r_out = P.res("out")
        for fn, args in ((phase_diff_attn, (odT, r_scr)), (phase_swa, (osT, r_scr)),
                         (phase_post, (odT, osT, x1s, x1T, r_scr)), (phase_moe, (x1s, x1T, x2, r_out))):
            with ExitStack() as st2:
                P.stack = st2
                fn(P, nc, T, ps, psT, r_ps, r_psT, *args)
                P.barrier()
        P.stack = stack
        P.finish()
    return nc


_NC_CACHE = {}


def _get(name, fn):
    if name not in _NC_CACHE:
        _NC_CACHE[name] = fn()
    return _NC_CACHE[name]


def _own_rows(arr_b, r):
    return arr_b.reshape(NBLK, 4, 128, -1)[:, r].reshape(TOK, -1)


def kernel(**inp):
    f32 = np.float32
    ncA = _get("A", build_A)
    ncB = _get("B", build_B)
    cores = list(range(NCORES))
    xcur = [np.ascontiguousarray(_own_rows(np.asarray(inp["x"][c // 4], f32), c % 4)) for c in cores]
    for L in range(DEPTH):
        w_in = np.ascontiguousarray(inp["w_in"][L], f32)
        b_in = np.asarray(inp["b_in"][L], f32)
        bcol = np.ascontiguousarray(b_in.reshape(50, 128).T)
        xT = [np.ascontiguousarray(xc.T) for xc in xcur]
        mapsA = [{"xT": xT[c], "w_in": w_in, "bcol": bcol, "b_in": b_in[None, :]} for c in cores]
        resA = run_bass_kernel_spmd(ncA, mapsA, core_ids=cores).results
        kd_all, vd_all, ks_all, vs_all = [], [], [], []
        for b in range(BATCH):
            rs = [resA[4 * b + r] for r in range(4)]
            kd_all.append(np.ascontiguousarray(
                np.stack([x_["kd"].reshape(8, 128, NBLK, 128) for x_ in rs], axis=3).reshape(8, 128, SEQ)))
            vd_all.append(np.ascontiguousarray(
                np.stack([x_["vd"].reshape(NBLK, 128, 1024) for x_ in rs], axis=1).reshape(SEQ, 1024)))
            ks_all.append(np.stack([x_["ks"].reshape(128, NBLK, 128) for x_ in rs], axis=2).reshape(128, NCH, 128))
            vs_all.append(np.stack([x_["vs"].reshape(NBLK, 128, 128) for x_ in rs], axis=1).reshape(NCH, 128, 128))
        mapsB = []
        for c in cores:
            b, r = c // 4, c % 4
            blk_cur = np.arange(NBLK) * 4 + r
            blk_prev = blk_cur - 1
            ksb = ks_all[b]
            ks_cur = ksb[:, blk_cur, :].reshape(2, 64, TOK)
            ks_prev = ksb[:, np.maximum(blk_prev, 0), :].copy()
            vs_prev = vs_all[b][np.maximum(blk_prev, 0)].copy()
            if blk_prev[0] < 0:
                ks_prev[:, 0, :] = 0
                vs_prev[0] = 0
            ks_prev = ks_prev.reshape(2, 64, TOK)
            m = {
                "qd": resA[c]["qd"], "kd_all": kd_all[b], "vd_all": vd_all[b], "qs": resA[c]["qs"],
                "ks2_cur": np.ascontiguousarray(np.concatenate([ks_cur, ks_cur], axis=1)),
                "ks2_prev": np.ascontiguousarray(np.concatenate([ks_prev, ks_prev], axis=1)),
                "vs_cur": np.ascontiguousarray(vs_all[b][blk_cur].reshape(TOK, 128)),
                "vs_prev": np.ascontiguousarray(vs_prev.reshape(TOK, 128)),
                "w_in": w_in, "bcol": bcol, "xT": xT[c], "x": xcur[c],
                "pT": np.ascontiguousarray(_own_rows(np.asarray(inp["p"][L][b], f32), r).T),
                "bgu_col": np.ascontiguousarray(
                    np.asarray(inp["b_gate_up"][L], f32).reshape(E, 16, 128).transpose(2, 0, 1).reshape(128, E * 16)),
            }
            m.update(host_consts(r, L))
            m.update(host_consts_swa(r))
            for nm in ("lambda_q1", "lambda_k1", "lambda_q2", "lambda_k2", "subln_w", "sinks", "b_out", "ln1_g", "ln1_b",
                       "b_router", "ln2_g", "ln2_b"):
                m[nm] = np.ascontiguousarray(np.asarray(inp[nm][L], f32)[None, :])
            for nm in ("w_br_diff", "w_br_swa", "w_out", "w_router", "b_down", "w_gate_up", "w_down", "w_ple_gate", "w_ple_proj"):
                m[nm] = np.ascontiguousarray(inp[nm][L], f32)
            mapsB.append(m)
        resB = run_bass_kernel_spmd(ncB, mapsB, core_ids=cores).results
        xcur = [resB[c]["x2"] for c in cores]
    out = np.zeros((BATCH, SEQ, D), f32)
    for c in cores:
        b, r = c // 4, c % 4
        out[b].reshape(NBLK, 4, 128, D)[:, r] = xcur[c].reshape(NBLK, 128, D)
    return out
```

```python
import math
from contextlib import ExitStack

import numpy as np
import ml_dtypes
import concourse.bass as bass
import concourse.mybir as mybir
from concourse.bass_utils import run_bass_kernel_spmd

F32 = mybir.dt.float32
BF16 = mybir.dt.bfloat16
AF = mybir.ActivationFunctionType
ALU = mybir.AluOpType
NPBF = ml_dtypes.bfloat16

D = 1024
BATCH = 2
SEQ = 16384
DEPTH = 2
NCORES = 8
NBLK = 32
TOK = NBLK * 128
NCH = SEQ // 128
E = 32
LN_EPS = 1e-5
RMS_EPS = 1e-5
DN_ALPHA = (2 * DEPTH) ** 0.25
CFG = {
    "selfsync": True,
    "alibi_thr": 150.0,
}


class Sem:
    def __init__(self, h):
        self.h = h
        self.count = 0


class Res:
    def __init__(self, name, dsem=None):
        self.name = name
        self.w = None
        self.r = {}
        self.dsem = dsem


class Eng:
    def __init__(self, name, sem, selfsync):
        self.name = name
        self.sem = sem
        self.selfsync = selfsync
        self.waited = {}
        self.ops = []


class Prog:
    def __init__(self, nc, stack):
        self.nc = nc
        self.stack = stack
        self.allsems = []
        self.engs = {}
        for name, selfsync in (("pe", False), ("act", True), ("dve", True), ("pool", True), ("sp", False)):
            self.engs[name] = Eng(name, self.new_sem("e_" + name), selfsync and CFG["selfsync"])
        self.nres = 0
        self.uid = 0
        self.dpool = [self.new_sem("dp%d" % i) for i in range(48)]
        self.dpi = 0

    def phase_begin(self):
        self.dpi = 0

    def new_sem(self, name):
        s = Sem(self.stack.enter_context(self.nc.semaphore(name)))
        self.allsems.append(s)
        return s

    def res(self, name=None, dma=False):
        self.nres += 1
        name = name or ("r%d" % self.nres)
        d = None
        if dma:
            d = self.dpool[self.dpi]
            self.dpi += 1
        return Res(name, d)

    def sbuf(self, name, shape, dtype):
        self.uid += 1
        return self.stack.enter_context(self.nc.sbuf_tensor("%s_u%d" % (name, self.uid), list(shape), dtype))

    def psum(self, name, shape, dtype):
        self.uid += 1
        return self.stack.enter_context(self.nc.psum_tensor("%s_u%d" % (name, self.uid), list(shape), dtype))

    def emit(self, eng, fn, reads=(), writes=(), dsem=None, xreads=()):
        e = self.engs[eng]
        need = {}

        def add(s, v):
            if need.get(s, 0) < v:
                need[s] = v

        for r in reads:
            if r.w is not None:
                add(*r.w)
        for r in xreads:
            if r.w is not None:
                add(*r.w)
            for s, v in r.r.items():
                if s is not e.sem:
                    add(s, v)
        reads = list(reads) + list(xreads)
        for w in writes:
            if w.w is not None:
                add(*w.w)
            for s, v in w.r.items():
                add(s, v)
        for s, v in need.items():
            if s is e.sem and not e.selfsync:
                continue
            if e.waited.get(s, 0) >= v:
                continue
            e.waited[s] = v
            e.ops.append(("wait", s, v))
        if dsem is not None:
            dsem.count += 16
            ev = (dsem, dsem.count)
            e.ops.append(("op", fn, dsem, 16))
        else:
            e.sem.count += 1
            ev = (e.sem, e.sem.count)
            e.ops.append(("op", fn, e.sem, 1))
        for r in reads:
            if r.r.get(ev[0], 0) < ev[1]:
                r.r[ev[0]] = ev[1]
        for w in writes:
            w.w = ev
            w.r = {}
        return ev

    def dma(self, eng, out, in_, reads, writes, sem_res, **kw):
        self.emit(eng, lambda h: h.dma_start(out=out, in_=in_, **kw), reads, writes, dsem=sem_res.dsem)

    def barrier(self):
        for e in self.engs.values():
            for s in self.allsems:
                if s is e.sem:
                    continue
                if s.count > e.waited.get(s, 0):
                    e.waited[s] = s.count
                    e.ops.append(("wait", s, s.count))

    def finish(self, final_eng="sp"):
        e = self.engs[final_eng]
        for s in self.allsems:
            if s is e.sem:
                continue
            if s.count > e.waited.get(s, 0):
                e.waited[s] = s.count
                e.ops.append(("wait", s, s.count))
        nc = self.nc
        with nc.Block() as block:
            def replay(eng, h):
                for op in eng.ops:
                    if op[0] == "wait":
                        h.wait_ge(op[1].h, op[2])
                    else:
                        op[1](h).then_inc(op[2].h, op[3])

            @block.tensor
            def _(h):
                replay(self.engs["pe"], h)

            @block.scalar
            def _(h):
                replay(self.engs["act"], h)

            @block.vector
            def _(h):
                replay(self.engs["dve"], h)

            @block.gpsimd
            def _(h):
                replay(self.engs["pool"], h)

            @block.sync
            def _(h):
                replay(self.engs["sp"], h)


AXX = mybir.AxisListType.X
GQ = 4


def diff_slopes():
    return np.array([2.0 ** (-(h + 1)) for h in range(8)], np.float64)


def swa_slopes():
    return np.array([2.0 ** (-8.0 * (h + 1) / 16.0) for h in range(16)], np.float64)


def host_consts(r, layer):
    sk = np.arange(128, dtype=np.float64)
    ab = np.zeros((128, 8, 128), np.float64)
    for h, m in enumerate(diff_slopes()):
        for dp in range(128):
            dij = dp - 3 + r
            ab[:, h, dp] = -30.0 if dij < 0 else np.maximum(m * (sk - 64.0 - 128.0 * dij), -20000.0)
    diag = (sk[:, None] <= sk[None, :]).astype(np.float32)
    m4 = np.zeros((128, 4, 2, GQ, 128), np.float32)
    for dp in range(4):
        c = 3 - dp
        t = np.ones((128, 128), np.float32) if c < r else (diag if c == r else np.zeros((128, 128), np.float32))
        m4[:, dp, :, :, :] = t[:, None, None, :]
    lam_init = 0.8 - 0.6 * math.exp(-0.3 * layer)
    lconst = np.zeros((128, 4), np.float32)
    lconst[:, 0] = lam_init
    lconst[:, 1] = 1.0 - lam_init
    lconst[:, 2] = -0.5
    return {
        "abias": ab.reshape(128, 1024).astype(np.float32),
        "mask4": m4.reshape(128, 4 * 2 * GQ * 128),
        "ident": np.eye(128, dtype=np.float32),
        "lconst": lconst,
    }


def phase_diff_attn(P, nc, T, ps, psT, r_ps, r_psT, odT, r_odT, kmap=lambda j: j):
    G = GQ
    Kt = [P.sbuf("Kt%d" % s, [128, SEQ], BF16) for s in range(2)]
    Vt = [P.sbuf("Vt%d" % s, [128, NCH, 129], BF16) for s in range(2)]
    Qt = [P.sbuf("Qt%d" % s, [128, TOK], BF16) for s in range(2)]
    PT = [P.sbuf("PT%d" % s, [128, 2, G * 128], BF16) for s in range(3)]
    osb = [P.sbuf("osb%d" % s, [128, 2, 129], F32) for s in range(2)]
    oTh = [P.sbuf("oTh%d" % s, [128, TOK], BF16) for s in range(2)]
    abias = P.sbuf("abias_t", [128, 1024], F32)
    mask4 = P.sbuf("mask4_t", [128, 4, 2, G * 128], BF16)
    ident = P.sbuf("identb", [128, 128], BF16)
    lconst = P.sbuf("lconst_t", [128, 4], F32)
    lamv = P.sbuf("lamv", [128, 4, 64], F32)
    lamt = P.sbuf("lamt", [128, 2, 64], F32)
    lams = P.sbuf("lams", [128, 4], F32)
    negl = P.sbuf("negl", [128, 1], F32)
    wsub = P.sbuf("wsub", [128, 128], F32)
    sm = [P.sbuf("sm%d" % s, [128, 8], F32) for s in range(2)]
    otmp = [P.sbuf("otmp%d" % s, [128, 128], F32) for s in range(2)]
    ofin = [P.sbuf("ofin%d" % s, [128, 128], F32) for s in range(2)]
    ojunk = P.sbuf("ojunk", [128, 128], F32)
    obf = [P.sbuf("obf%d" % s, [128, 128], BF16) for s in range(2)]

    r_K = [P.res("K%d" % s, dma=True) for s in range(2)]
    r_V = [P.res("V%d" % s, dma=True) for s in range(2)]
    r_Q = [P.res("Q%d" % s, dma=True) for s in range(2)]
    r_PT = [P.res("PT%d" % s) for s in range(3)]
    r_osb = [P.res("osb%d" % s) for s in range(2)]
    r_oTh = [P.res("oTh%d" % s, dma=True) for s in range(2)]
    r_c = P.res("dconst", dma=True)
    r_c2 = P.res("dconst2", dma=True)
    r_lam = P.res("lam")
    r_sm = [P.res("sm%d" % s) for s in range(2)]
    r_ot = [P.res("ot%d" % s) for s in range(2)]
    r_of = [P.res("of%d" % s) for s in range(2)]
    r_oj = P.res("oj")
    r_obf = [P.res("obf%d" % s) for s in range(2)]
    r_S = [r_ps[0:2], r_ps[2:4]]

    def oreg(g, c):
        q = 2 * g + c
        return 4 + q // 3, (q % 3) * 160

    P.dma("sp", abias[:], T["abias"], [], [r_c], r_c)
    P.dma("sp", lconst[:], T["lconst"], [], [r_c], r_c)
    for i, nm in enumerate(("lambda_q1", "lambda_k1", "lambda_q2", "lambda_k2")):
        P.dma("sp", lamv[:, i, :], T[nm].to_broadcast([128, 64]), [], [r_c], r_c)
    P.dma("sp", wsub[:], T["subln_w"].to_broadcast([128, 128]), [], [r_c], r_c)
    P.dma("pool", mask4[:].rearrange("p a b c -> p (a b c)"), T["mask4"], [], [r_c2], r_c2)
    P.dma("pool", ident[:], T["ident"], [], [r_c2], r_c2)
    for s in range(2):
        P.emit("dve", lambda h, s=s: h.memset(Vt[s][:, :, 128:129], 1.0), [], [r_V[s]])
    P.emit("dve", lambda h: h.tensor_tensor(out=lamt[:, 0, :], in0=lamv[:, 0, :], in1=lamv[:, 1, :], op=ALU.mult), [r_c], [r_lam])
    P.emit("dve", lambda h: h.tensor_tensor(out=lamt[:, 1, :], in0=lamv[:, 2, :], in1=lamv[:, 3, :], op=ALU.mult), [r_c], [r_lam])
    P.emit("dve", lambda h: h.tensor_reduce(out=lams[:, 0:2], in_=lamt[:], axis=AXX, op=ALU.add), [r_lam], [r_lam])
    P.emit("act", lambda h: h.activation(out=lams[:, 2:4], in_=lams[:, 0:2], func=AF.Exp), [r_lam], [r_lam])
    P.emit("dve", lambda h: h.tensor_tensor(out=lams[:, 0:1], in0=lams[:, 2:3], in1=lams[:, 3:4], op=ALU.subtract), [r_lam], [r_lam])
    P.emit("dve", lambda h: h.tensor_scalar(out=negl[:], in0=lams[:, 0:1], scalar1=lconst[:, 0:1], scalar2=-1.0,
                                            op0=ALU.add, op1=ALU.mult), [r_lam, r_c], [r_lam])
    P.emit("dve", lambda h: h.tensor_scalar(out=wsub[:], in0=wsub[:], scalar1=lconst[:, 1:2], scalar2=None, op0=ALU.mult),
           [r_c], [r_c])
    for bnk in range(4, 7):
        P.emit("dve", lambda h, bnk=bnk: h.memset(ps[:, bnk, :], 0.0), [], [r_ps[bnk]])

    kv = T["kd_all"]
    vv = T["vd_all"].rearrange("(j p) c -> p j c", p=128)
    cnt = {"pt": 0, "fin": 0}

    def load_head(h):
        s = h % 2
        for q4 in range(4):
            P.dma("sp", Kt[s][:, q4 * 4096:(q4 + 1) * 4096], kv[h][:, q4 * 4096:(q4 + 1) * 4096], [], [r_K[s]], r_K[s])
        for j8 in range(8):
            P.dma("sp", Vt[s][:, j8 * 16:(j8 + 1) * 16, 0:128], vv[:, j8 * 16:(j8 + 1) * 16, h * 128:(h + 1) * 128],
                  [], [r_V[s]], r_V[s])
        P.dma("sp", Qt[s][:], T["qd"][h], [], [r_Q[s]], r_Q[s])

    def finalize(h, g, n):
        hs = h % 2
        f = cnt["fin"] % 2
        cnt["fin"] += 1
        for c in range(2):
            bnk, off = oreg(g, c)
            P.emit("dve", lambda e, c=c, bnk=bnk, off=off: e.tensor_copy(out=osb[f][:, c, :], in_=ps[:, bnk, off:off + 129]),
                   [], [r_osb[f]], xreads=[r_ps[bnk]])
            P.emit("dve", lambda e, bnk=bnk, off=off: e.memset(ps[:, bnk, off:off + 129], 0.0), [], [r_ps[bnk]])
        s_ = sm[f]
        P.emit("dve", lambda e: e.reciprocal(out=s_[:, 0:2], in_=osb[f][:, :, 128]), [r_osb[f]], [r_sm[f]])
        P.emit("dve", lambda e: e.tensor_tensor(out=s_[:, 2:3], in0=s_[:, 1:2], in1=negl[:], op=ALU.mult), [r_sm[f], r_lam], [r_sm[f]])
        P.emit("dve", lambda e: e.tensor_scalar(out=otmp[f][:], in0=osb[f][:, 0, 0:128], scalar1=s_[:, 0:1], scalar2=None, op0=ALU.mult),
               [r_osb[f], r_sm[f]], [r_ot[f]])
        P.emit("dve", lambda e: e.scalar_tensor_tensor(out=ofin[f][:], in0=osb[f][:, 1, 0:128], scalar=s_[:, 2:3], in1=otmp[f][:],
                                                       op0=ALU.mult, op1=ALU.add), [r_osb[f], r_sm[f], r_ot[f]], [r_of[f]])
        P.emit("dve", lambda e: e.scalar_tensor_tensor(out=ojunk[:], in0=ofin[f][:], scalar=1.0, in1=ofin[f][:],
                                                       op0=ALU.mult, op1=ALU.mult, accum_out=s_[:, 3:4]), [r_of[f]], [r_oj, r_sm[f]])
        P.emit("dve", lambda e: e.tensor_scalar(out=s_[:, 4:5], in0=s_[:, 3:4], scalar1=1.0 / 128.0, scalar2=RMS_EPS,
                                                op0=ALU.mult, op1=ALU.add), [r_sm[f]], [r_sm[f]])
        P.emit("pool", lambda e: e.tensor_tensor(out=s_[:, 5:6], in0=s_[:, 4:5], in1=lconst[:, 2:3], op=ALU.pow), [r_sm[f], r_c], [r_sm[f]])
        P.emit("dve", lambda e: e.scalar_tensor_tensor(out=obf[f][:], in0=ofin[f][:], scalar=s_[:, 5:6], in1=wsub[:],
                                                       op0=ALU.mult, op1=ALU.mult), [r_of[f], r_sm[f], r_c], [r_obf[f]])
        P.emit("pe", lambda e: e.transpose(out=psT[:, 0:128], in_=obf[f][:], identity=ident[:]), [r_obf[f], r_c2], [r_psT])
        P.emit("dve", lambda e: e.tensor_copy(out=oTh[hs][:, n * 128:(n + 1) * 128], in_=psT[:, 0:128]), [], [r_oTh[hs]], xreads=[r_psT])

    load_head(0)
    for h in range(8):
        hs = h % 2
        if h + 1 < 8:
            load_head(h + 1)
        steps = []
        dpcap = 1 << 30
        if CFG["alibi_thr"] is not None:
            dpcap = 4
            while float(diff_slopes()[h]) * (128.0 * (dpcap - 3 - 1) + 1.0) <= CFG["alibi_thr"]:
                dpcap += 1
        lastdp = {}
        for m in range(NBLK // G):
            blocks = [G * m + (G - 1 - g) for g in range(G)]
            ndp = min(4 * blocks[0] + 4, dpcap)
            for n in blocks:
                lastdp[n] = min(4 * n + 3, dpcap - 1)
            for dp in range(ndp):
                act = [(g, n, 4 * n + 3 - dp) for g, n in enumerate(blocks) if dp <= lastdp[n]]
                steps.append((dp, act))

        def emit_S(t):
            dp, act = steps[t]
            sl = t % 2
            for (g, n, j) in act:
                for c in range(2):
                    P.emit("pe", lambda e, g=g, n=n, j=j, c=c, sl=sl, hs=hs: e.matmul(
                        ps[:, 2 * sl + c, g * 128:(g + 1) * 128],
                        lhsT=Kt[hs][c * 64:(c + 1) * 64, kmap(j) * 128:(kmap(j) + 1) * 128],
                        rhs=Qt[hs][c * 64:(c + 1) * 64, n * 128:(n + 1) * 128], start=True, stop=True),
                        [r_K[hs], r_Q[hs]], [r_S[sl][c]])
            na = len(act)
            pt = cnt["pt"] % 3
            cnt["pt"] += 1
            if CFG.get("dbg_noact"):
                return pt
            P.emit("act", lambda e, sl=sl, na=na, pt=pt, dp=dp, h=h: e.activation(
                out=PT[pt][:, :, 0:na * 128], in_=ps[:, 2 * sl:2 * sl + 2, 0:na * 128], func=AF.Exp,
                bias=abias[:, h * 128 + dp:h * 128 + dp + 1], scale=1.0),
                [r_c], [r_PT[pt]], xreads=r_S[sl])
            if dp < 4:
                P.emit("dve", lambda e, na=na, pt=pt, dp=dp: e.tensor_tensor(
                    out=PT[pt][:, :, 0:na * 128], in0=PT[pt][:, :, 0:na * 128], in1=mask4[:, dp, :, 0:na * 128], op=ALU.mult),
                    [r_c2], [r_PT[pt]])
            return pt

        def emit_PV(t, pt):
            dp, act = steps[t]
            for (g, n, j) in act:
                for c in range(2):
                    if CFG.get("dbg_nopv"):
                        continue
                    bnk, off = oreg(g, c)
                    P.emit("pe", lambda e, g=g, j=j, c=c, pt=pt, hs=hs, bnk=bnk, off=off: e.matmul(
                        ps[:, bnk, off:off + 129],
                        lhsT=PT[pt][:, c, g * 128:(g + 1) * 128], rhs=Vt[hs][:, kmap(j), :],
                        start=False, stop=False, skip_group_check=True),
                        [r_PT[pt], r_V[hs]], [r_ps[bnk]])
                if dp == lastdp[n]:
                    finalize(h, g, n)

        pts = {}
        for t in range(len(steps) + 1):
            if t < len(steps):
                pts[t] = emit_S(t)
            if t >= 1:
                emit_PV(t - 1, pts.pop(t - 1))
        P.dma("sp", odT[h * 128:(h + 1) * 128, :], oTh[hs][:], [r_oTh[hs]], [r_odT], r_oTh[hs])


def host_consts_swa(r):
    sk = np.arange(128, dtype=np.float64)
    sl = swa_slopes()
    sb = np.zeros((128, 16, 2), np.float64)
    for h in range(16):
        sb[:, h, 0] = sl[h] * (sk - 192.0)
        sb[:, h, 1] = sl[h] * (sk - 64.0)
    cur = (sk[:, None] <= sk[None, :]).astype(np.float32)
    prev = (sk[:, None] > sk[None, :]).astype(np.float32)
    prev0 = prev if r > 0 else np.zeros_like(prev)
    sm = np.zeros((128, 3, 2, 4, 128), np.float32)
    for i, t in enumerate((prev, cur, prev0)):
        sm[:, i] = t[:, None, None, :]
    sinkc = (sk[:, None] - 64.0) * sl[None, :]
    return {"sbias": sb.reshape(128, 32).astype(np.float32), "smask": sm.reshape(128, 3 * 1024),
            "sinkc": sinkc.astype(np.float32)}


def phase_swa(P, nc, T, ps, psT, r_ps, r_psT, osT, r_osT):
    qsT = P.sbuf("qsT", [128, 8, TOK], BF16)
    ksT = P.sbuf("ksT", [128, 2, 2, TOK], BF16)
    Vs = P.sbuf("Vs", [128, NBLK, 2, 2, 65], BF16)
    stash = P.sbuf("ostash", [128, 8, TOK], BF16)
    sbias = P.sbuf("sbias_t", [128, 32], F32)
    smask = P.sbuf("smask_t", [128, 3, 1024], BF16)
    ident = P.sbuf("identb2", [128, 128], BF16)
    sterm = P.sbuf("sterm", [128, 16], F32)
    sinkc = P.sbuf("sinkc_t", [128, 16], F32)
    PT = [P.sbuf("sPT%d" % s, [128, 2, 512], BF16) for s in range(3)]
    osb = [P.sbuf("sosb%d" % s, [128, 2, 4, 65], F32) for s in range(2)]
    lt = [P.sbuf("slt%d" % s, [128, 2, 4], F32) for s in range(2)]
    obf = [P.sbuf("sobf%d" % s, [128, 512], BF16) for s in range(2)]

    r_q = P.res("sq", dma=True)
    r_k = P.res("sk", dma=True)
    r_v = P.res("sv", dma=True)
    r_c = P.res("sc", dma=True)
    r_c2 = P.res("sc2", dma=True)
    r_st = P.res("sterm")
    r_PT = [P.res("sPT%d" % s) for s in range(3)]
    r_osb = [P.res("sosb%d" % s) for s in range(2)]
    r_lt = [P.res("slt%d" % s) for s in range(2)]
    r_obf = [P.res("sobf%d" % s) for s in range(2)]
    r_stash = P.res("stash", dma=True)
    r_S = [r_ps[0:2], r_ps[2:4]]
    r_O = r_ps[4:6]

    P.dma("sp", sbias[:], T["sbias"], [], [r_c], r_c)
    P.dma("sp", sinkc[:], T["sinkc"], [], [r_c], r_c)
    P.dma("sp", sterm[:], T["sinks"].to_broadcast([128, 16]), [], [r_c], r_c)
    P.dma("pool", smask[:].rearrange("p a b -> p (a b)"), T["smask"], [], [r_c2], r_c2)
    P.dma("pool", ident[:], T["ident"], [], [r_c2], r_c2)
    for m in range(8):
        P.dma("sp", qsT[:, m, :], T["qs"][m], [], [r_q], r_q)
    for role, nm in enumerate(("ks2_prev", "ks2_cur")):
        for g in range(2):
            P.dma("sp", ksT[:, role, g, :], T[nm][g], [], [r_k], r_k)
    P.emit("dve", lambda e: e.memset(Vs[:, :, :, :, 64:65], 1.0), [], [r_v])
    for role, nm in enumerate(("vs_prev", "vs_cur")):
        src = T[nm].rearrange("(n p) (g d) -> p n g d", p=128, g=2)
        for g in range(2):
            P.dma("sp", Vs[:, :, role, g, 0:64], src[:, :, g, :], [], [r_v], r_v)
    P.emit("dve", lambda e: e.tensor_tensor(out=sterm[:], in0=sterm[:], in1=sinkc[:], op=ALU.add), [r_c], [r_st])
    P.emit("act", lambda e: e.activation(out=sterm[:], in_=sterm[:], func=AF.Exp), [r_st], [r_st])
    for b in range(2):
        P.emit("dve", lambda e, b=b: e.memset(ps[:, 4 + b, :], 0.0), [], [r_O[b]])

    cnt = {"pt": 0, "fin": 0, "step": 0}
    pending = []

    def emit_S(n, g, role):
        sl = cnt["step"] % 2
        cnt["step"] += 1
        pt = cnt["pt"] % 3
        cnt["pt"] += 1
        for a in range(4):
            for par in range(2):
                m = g * 4 + a
                P.emit("pe", lambda e, a=a, par=par, m=m, sl=sl: e.matmul(
                    ps[:, 2 * sl + par, a * 128:(a + 1) * 128],
                    lhsT=ksT[par * 64:(par + 1) * 64, role, g, n * 128:(n + 1) * 128],
                    rhs=qsT[par * 64:(par + 1) * 64, m, n * 128:(n + 1) * 128], start=True, stop=True),
                    [r_k, r_q], [r_S[sl][par]])
        for a in range(4):
            for par in range(2):
                hh = g * 8 + 2 * a + par
                P.emit("act", lambda e, a=a, par=par, hh=hh, sl=sl, pt=pt: e.activation(
                    out=PT[pt][:, par, a * 128:(a + 1) * 128], in_=ps[:, 2 * sl + par, a * 128:(a + 1) * 128],
                    func=AF.Exp, bias=sbias[:, hh * 2 + role:hh * 2 + role + 1], scale=1.0),
                    [r_c], [r_PT[pt]], xreads=[r_S[sl][par]])
        mi = 1 if role == 1 else (2 if n == 0 else 0)
        P.emit("dve", lambda e, pt=pt, mi=mi: e.tensor_tensor(
            out=PT[pt][:].rearrange("p a b -> p (a b)"), in0=PT[pt][:].rearrange("p a b -> p (a b)"),
            in1=smask[:, mi, :], op=ALU.mult), [r_c2], [r_PT[pt]])
        return pt

    def emit_PV(n, g, role, pt):
        for a in range(4):
            for par in range(2):
                P.emit("pe", lambda e, a=a, par=par, pt=pt: e.matmul(
                    ps[:, 4 + par, a * 65:(a + 1) * 65], lhsT=PT[pt][:, par, a * 128:(a + 1) * 128],
                    rhs=Vs[:, n, role, g, :], start=False, stop=False, skip_group_check=True),
                    [r_PT[pt], r_v], [r_O[par]])
        if role == 1:
            f = cnt["fin"] % 2
            cnt["fin"] += 1
            for par in range(2):
                P.emit("dve", lambda e, par=par: e.tensor_copy(
                    out=osb[f][:, par, :, :].rearrange("p a d -> p (a d)"), in_=ps[:, 4 + par, 0:260]),
                    [], [r_osb[f]], xreads=[r_O[par]])
                P.emit("dve", lambda e, par=par: e.memset(ps[:, 4 + par, 0:260], 0.0), [], [r_O[par]])
            stv = sterm[:, g * 8:(g + 1) * 8].rearrange("p (a two) -> p two a", two=2)
            P.emit("dve", lambda e: e.tensor_tensor(out=lt[f][:], in0=osb[f][:, :, :, 64], in1=stv, op=ALU.add),
                   [r_osb[f], r_st], [r_lt[f]])
            P.emit("dve", lambda e: e.reciprocal(out=lt[f][:], in_=lt[f][:]), [r_lt[f]], [r_lt[f]])
            for a in range(4):
                for par in range(2):
                    hl = 2 * a + par
                    P.emit("dve", lambda e, a=a, par=par, hl=hl: e.tensor_scalar(
                        out=obf[f][:, hl * 64:(hl + 1) * 64], in0=osb[f][:, par, a, 0:64], scalar1=lt[f][:, par, a:a + 1],
                        scalar2=None, op0=ALU.mult), [r_osb[f], r_lt[f]], [r_obf[f]])
            for k in range(4):
                P.emit("pe", lambda e, k=k: e.transpose(out=psT[:, k * 128:(k + 1) * 128], in_=obf[f][:, k * 128:(k + 1) * 128],
                                                        identity=ident[:]), [r_obf[f], r_c2], [r_psT])
            P.emit("dve", lambda e: e.tensor_copy(
                out=stash[:, g * 4:(g + 1) * 4, n * 128:(n + 1) * 128],
                in_=psT[:, 0:512].rearrange("p (k t) -> p k t", k=4)), [], [r_stash], xreads=[r_psT])

    seq = [(n, g, role) for n in range(NBLK) for g in range(2) for role in range(2)]
    prev = None
    for item in seq:
        pt = emit_S(*item)
        if prev is not None:
            emit_PV(*prev)
        prev = item + (pt,)
    emit_PV(*prev)
    for k in range(8):
        P.dma("sp", osT[k * 128:(k + 1) * 128, :], stash[:, k, :], [r_stash], [r_osT], r_stash)


def layer_norm_tok(P, y, out, gam, bet, r_y, r_out, r_gb, tmp, lconst, r_lc):
    st, mv = tmp["st"], tmp["mv"]
    r_t = tmp["r"]
    for c in range(2):
        P.emit("dve", lambda e, c=c: e.bn_stats(out=st[:, c, :], in_=y[:, c * 512:(c + 1) * 512]), [r_y], [r_t])
    P.emit("dve", lambda e: e.bn_aggr(out=mv[:, 0:2], in_=st[:].rearrange("p a b -> p (a b)")), [r_t], [r_t])
    P.emit("dve", lambda e: e.tensor_scalar(out=mv[:, 2:3], in0=mv[:, 1:2], scalar1=LN_EPS, scalar2=None, op0=ALU.add), [r_t], [r_t])
    P.emit("pool", lambda e: e.tensor_tensor(out=mv[:, 3:4], in0=mv[:, 2:3], in1=lconst[:, 2:3], op=ALU.pow), [r_t, r_lc], [r_t])
    P.emit("dve", lambda e: e.tensor_scalar(out=y[:], in0=y[:], scalar1=mv[:, 0:1], scalar2=mv[:, 3:4],
                                            op0=ALU.subtract, op1=ALU.mult), [r_t], [r_y])
    P.emit("pool", lambda e: e.tensor_tensor(out=y[:], in0=y[:], in1=gam[:], op=ALU.mult), [r_gb], [r_y])
    P.emit("dve", lambda e: e.tensor_tensor(out=out[:], in0=y[:], in1=bet[:], op=ALU.add), [r_y, r_gb], [r_out])


def phase_post(P, nc, T, ps, psT, r_ps, r_psT, odT, osT, x1s, x1T, r_x1):
    Wg = P.sbuf("Wg", [128, 8, 2048], BF16)
    Wa = P.sbuf("Wa", [128, 8, 1024], BF16)
    Wb = P.sbuf("Wb", [128, 8, 1024], BF16)
    Wo = P.sbuf("Wo", [128, 8, 1024], BF16)
    bcol = P.sbuf("bcol3", [128, 50], F32)
    lconst = P.sbuf("lconst3", [128, 4], F32)
    ident = P.sbuf("identb3", [128, 128], BF16)
    bo = P.sbuf("bo_bc", [128, 1024], F32)
    g1 = P.sbuf("g1_bc", [128, 1024], F32)
    b1 = P.sbuf("b1_bc", [128, 1024], F32)
    xTb = [P.sbuf("xTb3_%d" % s, [128, 8, 512], BF16) for s in range(2)]
    odb = [P.sbuf("odb%d" % s, [128, 8, 512], BF16) for s in range(2)]
    osb = [P.sbuf("osb3_%d" % s, [128, 8, 512], BF16) for s in range(2)]
    sg = [P.sbuf("sg%d" % s, [128, 2, 512], F32) for s in range(2)]
    t12 = [P.sbuf("t12_%d" % s, [128, 2, 512], F32) for s in range(2)]
    mT = P.sbuf("mT", [128, 8, 512], BF16)
    xin = [P.sbuf("xin%d" % s, [128, 1024], F32) for s in range(2)]
    ysb = [P.sbuf("ysb%d" % s, [128, 1024], F32) for s in range(2)]
    x1o = [P.sbuf("x1o%d" % s, [128, 1024], F32) for s in range(2)]
    x1b = [P.sbuf("x1b%d" % s, [128, 1024], BF16) for s in range(2)]
    x1Tb = [P.sbuf("x1Tb%d" % s, [128, 8, 128], BF16) for s in range(2)]
    lnt = [{"st": P.sbuf("lnst%d" % s, [128, 2, 6], F32), "mv": P.sbuf("lnmv%d" % s, [128, 4], F32), "r": P.res("lnr%d" % s)}
           for s in range(2)]

    r_w = P.res("w3", dma=True)
    r_c = P.res("c3", dma=True)
    r_xT = [P.res("xT3_%d" % s, dma=True) for s in range(2)]
    r_od = [P.res("od3_%d" % s, dma=True) for s in range(2)]
    r_os = [P.res("os3_%d" % s, dma=True) for s in range(2)]
    r_sg = [P.res("sg%d" % s) for s in range(2)]
    r_t12 = [P.res("t12_%d" % s) for s in range(2)]
    r_mT = P.res("mT")
    r_xin = [P.res("xin%d" % s, dma=True) for s in range(2)]
    r_ysb = [P.res("ysb%d" % s) for s in range(2)]
    r_x1o = [P.res("x1o%d" % s, dma=True) for s in range(2)]
    r_x1b = [P.res("x1b%d" % s) for s in range(2)]
    r_x1Tb = [P.res("x1Tb%d" % s, dma=True) for s in range(2)]

    wv = T["w_in"].rearrange("(kc p) n -> p kc n", p=128)
    for k in range(8):
        P.dma("pool", Wg[:, k, :], wv[:, k, 4352:6400], [], [r_w], r_w)
    for Wt, nm in ((Wa, "w_br_diff"), (Wb, "w_br_swa"), (Wo, "w_out")):
        v = T[nm].rearrange("(kc p) n -> p kc n", p=128)
        for k2 in range(2):
            P.dma("pool", Wt[:, k2 * 4:(k2 + 1) * 4, :], v[:, k2 * 4:(k2 + 1) * 4, :], [], [r_w], r_w)
    P.dma("pool", ident[:], T["ident"], [], [r_w], r_w)
    P.dma("sp", bcol[:], T["bcol"], [], [r_c], r_c)
    P.dma("sp", lconst[:], T["lconst"], [], [r_c], r_c)
    P.dma("sp", bo[:], T["b_out"].to_broadcast([128, 1024]), [], [r_c], r_c)
    P.dma("sp", g1[:], T["ln1_g"].to_broadcast([128, 1024]), [], [r_c], r_c)
    P.dma("sp", b1[:], T["ln1_b"].to_broadcast([128, 1024]), [], [r_c], r_c)

    xTv = T["xT"].rearrange("(kc p) t -> p kc t", p=128)
    odv = odT.rearrange("(kc p) t -> p kc t", p=128)
    osv = osT.rearrange("(kc p) t -> p kc t", p=128)
    it = 0
    nb = 0
    for tg in range(8):
        s = tg % 2
        tsl = slice(tg * 512, (tg + 1) * 512)
        P.dma("pool", xTb[s][:], xTv[:, :, tsl], [], [r_xT[s]], r_xT[s])
        P.dma("sp", odb[s][:], odv[:, :, tsl], [], [r_od[s]], r_od[s])
        P.dma("sp", osb[s][:], osv[:, :, tsl], [], [r_os[s]], r_os[s])
        for f in range(8):
            fs = f % 2
            banks = [(it * 4 + j) % 4 for j in range(4)] if False else [0, 1, 2, 3]
            it += 1
            for j, (Wt, c0, src, rs) in enumerate(((Wg, f * 128, xTb[s], r_xT[s]), (Wg, 1024 + f * 128, xTb[s], r_xT[s]),
                                                   (Wa, f * 128, odb[s], r_od[s]), (Wb, f * 128, osb[s], r_os[s]))):
                for k in range(8):
                    P.emit("pe", lambda e, j=j, Wt=Wt, c0=c0, src=src, k=k: e.matmul(
                        ps[:, banks[j], :], lhsT=Wt[:, k, c0:c0 + 128], rhs=src[:, k, :], start=(k == 0), stop=(k == 7)),
                        [r_w, rs], [r_ps[banks[j]]])
            for j in range(2):
                P.emit("act", lambda e, j=j, fs=fs, f=f: e.activation(
                    out=sg[fs][:, j, :], in_=ps[:, banks[j], :], func=AF.Sigmoid,
                    bias=bcol[:, 34 + 8 * j + f:35 + 8 * j + f], scale=1.0), [r_c], [r_sg[fs]], xreads=[r_ps[banks[j]]])
            for j in range(2):
                P.emit("dve", lambda e, j=j, fs=fs: e.tensor_tensor(
                    out=t12[fs][:, j, :], in0=sg[fs][:, j, :], in1=ps[:, banks[2 + j], :], op=ALU.mult),
                    [r_sg[fs]], [r_t12[fs]], xreads=[r_ps[banks[2 + j]]])
            P.emit("pool", lambda e, fs=fs, f=f: e.tensor_tensor(
                out=mT[:, f, :], in0=t12[fs][:, 0, :], in1=t12[fs][:, 1, :], op=ALU.add), [r_t12[fs]], [r_mT])
        for bi in range(4):
            n = tg * 4 + bi
            bs = nb % 2
            nb += 1
            P.dma("sp", xin[bs][:], T["x"][n * 128:(n + 1) * 128, :], [], [r_xin[bs]], r_xin[bs])
            for half in range(2):
                bank = 4 + half
                for k in range(8):
                    P.emit("pe", lambda e, k=k, bi=bi, half=half, bank=bank: e.matmul(
                        ps[:, bank, :], lhsT=mT[:, k, bi * 128:(bi + 1) * 128], rhs=Wo[:, k, half * 512:(half + 1) * 512],
                        start=(k == 0), stop=(k == 7)), [r_mT, r_w], [r_ps[bank]])
                P.emit("dve", lambda e, half=half, bank=bank, bs=bs: e.tensor_tensor(
                    out=ysb[bs][:, half * 512:(half + 1) * 512], in0=ps[:, bank, :], in1=bo[:, half * 512:(half + 1) * 512],
                    op=ALU.add), [r_c], [r_ysb[bs]], xreads=[r_ps[bank]])
            P.emit("dve", lambda e, bs=bs: e.scalar_tensor_tensor(
                out=ysb[bs][:], in0=xin[bs][:], scalar=DN_ALPHA, in1=ysb[bs][:], op0=ALU.mult, op1=ALU.add),
                [r_xin[bs]], [r_ysb[bs]])
            layer_norm_tok(P, ysb[bs], x1o[bs], g1, b1, r_ysb[bs], r_x1o[bs], r_c, lnt[bs], lconst, r_c)
            P.dma("sp", x1s[n * 128:(n + 1) * 128, :], x1o[bs][:], [r_x1o[bs]], [r_x1], r_x1o[bs])
            P.emit("pool", lambda e, bs=bs: e.tensor_copy(out=x1b[bs][:], in_=x1o[bs][:]), [r_x1o[bs]], [r_x1b[bs]])
            for k in range(8):
                P.emit("pe", lambda e, k=k, bs=bs: e.transpose(out=psT[:, k * 128:(k + 1) * 128], in_=x1b[bs][:, k * 128:(k + 1) * 128],
                                                               identity=ident[:]), [r_x1b[bs], r_w], [r_psT])
            P.emit("dve", lambda e, bs=bs: e.tensor_copy(out=x1Tb[bs][:].rearrange("p k t -> p (k t)"), in_=psT[:, :]),
                   [], [r_x1Tb[bs]], xreads=[r_psT])
            P.dma("sp", x1T.rearrange("(kc p) t -> p kc t", p=128)[:, :, n * 128:(n + 1) * 128], x1Tb[bs][:],
                  [r_x1Tb[bs]], [r_x1], r_x1Tb[bs])


TG4 = 1024
NB4 = TG4 // 128


def phase_moe(P, nc, T, ps, psT, r_ps, r_psT, x1s, x1T, out_x2, r_out):
    Wgu = [P.sbuf("Wgu%d" % s, [128, 8, 2048], BF16) for s in range(2)]
    Wd = [P.sbuf("Wd%d" % s, [128, 8, 1024], BF16) for s in range(2)]
    x1Tb = P.sbuf("x1Tb4", [128, 8, TG4], BF16)
    pTb = P.sbuf("pTb4", [128, 2, TG4], BF16)
    acc = P.sbuf("acc4", [128, NB4, 1024], F32)
    actT = [P.sbuf("actT%d" % s, [128, 8, 512], BF16) for s in range(2)]
    tg_ = [P.sbuf("tg4_%d" % s, [128, 512], F32) for s in range(2)]
    ts_ = [P.sbuf("ts4_%d" % s, [128, 512], F32) for s in range(2)]
    tu_ = [P.sbuf("tu4_%d" % s, [128, 512], F32) for s in range(2)]
    Wr = P.sbuf("Wr4", [128, 8, 32], BF16)
    br = P.sbuf("br4", [128, 32], F32)
    bgu = P.sbuf("bgu4", [128, 32, 16], F32)
    bdn = P.sbuf("bdn4", [32, 1024], F32)
    lconst = P.sbuf("lconst4", [128, 4], F32)
    identf = P.sbuf("identf4", [128, 128], F32)
    g2 = P.sbuf("g2_bc", [128, 1024], F32)
    b2 = P.sbuf("b2_bc", [128, 1024], F32)
    lg = [P.sbuf("lg4_%d" % s, [128, 32], F32) for s in range(2)]
    rt = [P.sbuf("rt4_%d" % s, [128, 16], F32) for s in range(2)]
    msk = [P.sbuf("msk4_%d" % s, [128, 32], F32) for s in range(2)]
    ex = [P.sbuf("ex4_%d" % s, [128, 32], F32) for s in range(2)]
    Gt = P.sbuf("Gt4", [128, NB4, 32], F32)
    GT = [P.sbuf("GT4_%d" % s, [32, 128], F32) for s in range(2)]
    sgp = [P.sbuf("sgp4_%d" % s, [128, 512], F32) for s in range(2)]
    xin = [P.sbuf("xin4_%d" % s, [128, 1024], F32) for s in range(2)]
    lnt = [{"st": P.sbuf("lnst4_%d" % s, [128, 2, 6], F32), "mv": P.sbuf("lnmv4_%d" % s, [128, 4], F32), "r": P.res("lnr4_%d" % s)}
           for s in range(2)]

    r_W = [P.res("W4_%d" % s, dma=True) for s in range(2)]
    r_c = P.res("c4", dma=True)
    r_cw = P.res("cw4", dma=True)
    r_x1T = P.res("x1T4", dma=True)
    r_pT = P.res("pT4", dma=True)
    r_acc = [P.res("acc4_%d" % b, dma=True) for b in range(NB4)]
    r_actT = [P.res("actT%d" % s) for s in range(2)]
    r_tg = [P.res("tg4_%d" % s) for s in range(2)]
    r_ts = [P.res("ts4_%d" % s) for s in range(2)]
    r_tu = [P.res("tu4_%d" % s) for s in range(2)]
    r_lg = [P.res("lg4_%d" % s) for s in range(2)]
    r_rt = [P.res("rt4_%d" % s) for s in range(2)]
    r_G = P.res("G4")
    r_GT = [P.res("GT4_%d" % s) for s in range(2)]
    r_sgp = [P.res("sgp4_%d" % s) for s in range(2)]
    r_xin = [P.res("xin4_%d" % s, dma=True) for s in range(2)]

    P.dma("pool", Wr[:], T["w_router"].rearrange("(kc p) n -> p kc n", p=128), [], [r_cw], r_cw)
    P.dma("sp", br[:], T["b_router"].to_broadcast([128, 32]), [], [r_c], r_c)
    P.dma("sp", bgu[:].rearrange("p a b -> p (a b)"), T["bgu_col"], [], [r_c], r_c)
    P.dma("sp", bdn[:], T["b_down"], [], [r_c], r_c)
    P.dma("sp", lconst[:], T["lconst"], [], [r_c], r_c)
    P.dma("sp", identf[:], T["ident"], [], [r_c], r_c)
    P.dma("sp", g2[:], T["ln2_g"].to_broadcast([128, 1024]), [], [r_c], r_c)
    P.dma("sp", b2[:], T["ln2_b"].to_broadcast([128, 1024]), [], [r_c], r_c)
    P.emit("dve", lambda e: e.tensor_scalar(out=bgu[:, :, 8:16], in0=bgu[:, :, 8:16], scalar1=1.0, scalar2=None, op0=ALU.add),
           [r_c], [r_c])

    wguv = T["w_gate_up"].rearrange("e (kc p) n -> e p kc n", p=128)
    wdv = T["w_down"].rearrange("e (kc p) n -> e p kc n", p=128)
    x1Tv = x1T.rearrange("(kc p) t -> p kc t", p=128)
    pTv = T["pT"].rearrange("(kc p) t -> p kc t", p=128)

    def load_expert(e_, s):
        for k4 in range(4):
            P.dma("pool", Wgu[s][:, k4 * 2:(k4 + 1) * 2, :], wguv[e_][:, k4 * 2:(k4 + 1) * 2, :], [], [r_W[s]], r_W[s])
        for k2 in range(2):
            P.dma("pool", Wd[s][:, k2 * 4:(k2 + 1) * 4, :], wdv[e_][:, k2 * 4:(k2 + 1) * 4, :], [], [r_W[s]], r_W[s])

    cnt = {"w": 0, "el": 0, "y": 0, "fin": 0}
    for tq in range(TOK // TG4):
        tsl = slice(tq * TG4, (tq + 1) * TG4)
        P.dma("sp", x1Tb[:], x1Tv[:, :, tsl], [], [r_x1T], r_x1T)
        P.dma("pool", pTb[:], pTv[:, :, tsl], [], [r_pT], r_pT)
        ws0 = cnt["w"] % 2
        load_expert(0, ws0)
        for b in range(NB4):
            f = b % 2
            P.emit("pe", lambda e, b=b: [e.matmul(ps[:, 6, 0:32], lhsT=x1Tb[:, k, b * 128:(b + 1) * 128], rhs=Wr[:, k, :],
                                                  start=(k == 0), stop=(k == 7)) for k in range(8)][-1],
                   [r_x1T, r_cw], [r_ps[6]])
            P.emit("dve", lambda e, f=f: e.tensor_tensor(out=lg[f][:], in0=ps[:, 6, 0:32], in1=br[:], op=ALU.add),
                   [r_c], [r_lg[f]], xreads=[r_ps[6]])
            P.emit("dve", lambda e, f=f: e.max(out=rt[f][:, 0:8], in_=lg[f][:]), [r_lg[f]], [r_rt[f]])
            P.emit("dve", lambda e, f=f: e.tensor_scalar(out=msk[f][:], in0=lg[f][:], scalar1=rt[f][:, 3:4], scalar2=None, op0=ALU.is_ge),
                   [r_lg[f], r_rt[f]], [r_lg[f]])
            P.emit("dve", lambda e, f=f: e.tensor_scalar(out=rt[f][:, 8:9], in0=rt[f][:, 0:1], scalar1=-1.0, scalar2=None, op0=ALU.mult),
                   [r_rt[f]], [r_rt[f]])
            P.emit("act", lambda e, f=f: e.activation(out=ex[f][:], in_=lg[f][:], func=AF.Exp, bias=rt[f][:, 8:9], scale=1.0),
                   [r_lg[f], r_rt[f]], [r_lg[f]])
            P.emit("dve", lambda e, f=f: e.scalar_tensor_tensor(out=ex[f][:], in0=ex[f][:], scalar=1.0, in1=msk[f][:], op0=ALU.mult,
                                                                op1=ALU.mult, accum_out=rt[f][:, 9:10]), [r_lg[f]], [r_lg[f], r_rt[f]])
            P.emit("dve", lambda e, f=f: e.reciprocal(out=rt[f][:, 10:11], in_=rt[f][:, 9:10]), [r_rt[f]], [r_rt[f]])
            P.emit("dve", lambda e, f=f, b=b: e.tensor_scalar(out=Gt[:, b, :], in0=ex[f][:], scalar1=rt[f][:, 10:11], scalar2=None, op0=ALU.mult),
                   [r_lg[f], r_rt[f]], [r_G])
            P.emit("pe", lambda e, b=b: e.transpose(out=ps[0:32, 6, 128:256], in_=Gt[:, b, :], identity=identf[:]), [r_G, r_c], [r_ps[6]])
            P.emit("dve", lambda e, f=f: e.tensor_copy(out=GT[f][:], in_=ps[0:32, 6, 128:256]), [], [r_GT[f]], xreads=[r_ps[6]])
            for half in range(2):
                bank = 4 + half
                P.emit("pe", lambda e, f=f, half=half, bank=bank: e.matmul(
                    ps[:, bank, :], lhsT=GT[f][:], rhs=bdn[:, half * 512:(half + 1) * 512], start=True, stop=True),
                    [r_GT[f], r_c], [r_ps[bank]])
                P.emit("dve", lambda e, b=b, half=half, bank=bank: e.tensor_copy(
                    out=acc[:, b, half * 512:(half + 1) * 512], in_=ps[:, bank, :]), [], [r_acc[b]], xreads=[r_ps[bank]])
        for e_ in range(E):
            ws = cnt["w"] % 2
            cnt["w"] += 1
            if e_ + 1 < E:
                load_expert(e_ + 1, (ws + 1) % 2)
            for sgi in range(TG4 // 512):
                asl = cnt["el"] % 2
                for c in range(8):
                    es = cnt["el"] % 2
                    cnt["el"] += 1
                    bg, bu = 2 * es, 2 * es + 1
                    for bank, c0 in ((bg, c * 128), (bu, 1024 + c * 128)):
                        for k in range(8):
                            P.emit("pe", lambda e, bank=bank, c0=c0, k=k, ws=ws, sgi=sgi: e.matmul(
                                ps[:, bank, :], lhsT=Wgu[ws][:, k, c0:c0 + 128], rhs=x1Tb[:, k, sgi * 512:(sgi + 1) * 512],
                                start=(k == 0), stop=(k == 7)), [r_W[ws], r_x1T], [r_ps[bank]])
                    P.emit("act", lambda e, es=es, bg=bg, c=c, e_=e_: e.activation(
                        out=tg_[es][:], in_=ps[:, bg, :], func=AF.Identity, bias=bgu[:, e_, c:c + 1], scale=1.0),
                        [r_c], [r_tg[es]], xreads=[r_ps[bg]])
                    P.emit("pool", lambda e, es=es: e.tensor_scalar(out=tg_[es][:], in0=tg_[es][:], scalar1=7.0, scalar2=None, op0=ALU.min),
                           [], [r_tg[es]])
                    P.emit("act", lambda e, es=es: e.activation(out=ts_[es][:], in_=tg_[es][:], func=AF.Sigmoid, scale=1.702),
                           [r_tg[es]], [r_ts[es]])
                    P.emit("pool", lambda e, es=es: e.tensor_tensor(out=ts_[es][:], in0=tg_[es][:], in1=ts_[es][:], op=ALU.mult),
                           [r_tg[es]], [r_ts[es]])
                    P.emit("dve", lambda e, es=es, bu=bu, c=c, e_=e_: e.tensor_scalar(
                        out=tu_[es][:], in0=ps[:, bu, :], scalar1=bgu[:, e_, 8 + c:9 + c], scalar2=8.0, op0=ALU.add, op1=ALU.min),
                        [r_c], [r_tu[es]], xreads=[r_ps[bu]])
                    P.emit("dve", lambda e, es=es, c=c, asl=asl: e.scalar_tensor_tensor(
                        out=actT[asl][:, c, :], in0=tu_[es][:], scalar=-6.0, in1=ts_[es][:], op0=ALU.max, op1=ALU.mult),
                        [r_tu[es], r_ts[es]], [r_actT[asl]])
                for bi in range(4):
                    b = sgi * 4 + bi
                    for half in range(2):
                        bank = 4 + (cnt["y"] % 3)
                        cnt["y"] += 1
                        for c in range(8):
                            P.emit("pe", lambda e, c=c, bi=bi, half=half, bank=bank, ws=ws, asl=asl: e.matmul(
                                ps[:, bank, :], lhsT=actT[asl][:, c, bi * 128:(bi + 1) * 128],
                                rhs=Wd[ws][:, c, half * 512:(half + 1) * 512], start=(c == 0), stop=(c == 7)),
                                [r_actT[asl], r_W[ws]], [r_ps[bank]])
                        P.emit("dve", lambda e, b=b, half=half, bank=bank, e_=e_: e.scalar_tensor_tensor(
                            out=acc[:, b, half * 512:(half + 1) * 512], in0=ps[:, bank, :], scalar=Gt[:, b, e_:e_ + 1],
                            in1=acc[:, b, half * 512:(half + 1) * 512], op0=ALU.mult, op1=ALU.add),
                            [r_G], [r_acc[b]], xreads=[r_ps[bank]])
        ws = cnt["w"] % 2
        cnt["w"] += 1
        wpg = T["w_ple_gate"].rearrange("(kc p) n -> p kc n", p=128)
        wpp = T["w_ple_proj"].rearrange("(kc p) n -> p kc n", p=128)
        for k2 in range(2):
            P.dma("pool", Wgu[ws][:, k2 * 4:(k2 + 1) * 4, 0:1024], wpg[:, k2 * 4:(k2 + 1) * 4, :], [], [r_W[ws]], r_W[ws])
        P.dma("pool", Wd[ws][:, 0:2, :], wpp, [], [r_W[ws]], r_W[ws])
        for b in range(NB4):
            n = tq * NB4 + b
            f = cnt["fin"] % 2
            cnt["fin"] += 1
            P.dma("sp", xin[f][:], x1s[n * 128:(n + 1) * 128, :], [], [r_xin[f]], r_xin[f])
            for half in range(2):
                bg, bp = 2 * half, 2 * half + 1
                for k in range(8):
                    P.emit("pe", lambda e, k=k, b=b, half=half, bg=bg, ws=ws: e.matmul(
                        ps[:, bg, :], lhsT=x1Tb[:, k, b * 128:(b + 1) * 128], rhs=Wgu[ws][:, k, half * 512:(half + 1) * 512],
                        start=(k == 0), stop=(k == 7)), [r_x1T, r_W[ws]], [r_ps[bg]])
                for k in range(2):
                    P.emit("pe", lambda e, k=k, b=b, half=half, bp=bp, ws=ws: e.matmul(
                        ps[:, bp, :], lhsT=pTb[:, k, b * 128:(b + 1) * 128], rhs=Wd[ws][:, k, half * 512:(half + 1) * 512],
                        start=(k == 0), stop=(k == 1)), [r_pT, r_W[ws]], [r_ps[bp]])
                P.emit("act", lambda e, half=half, bg=bg: e.activation(out=sgp[half][:], in_=ps[:, bg, :], func=AF.Sigmoid),
                       [], [r_sgp[half]], xreads=[r_ps[bg]])
                P.emit("dve", lambda e, half=half, bp=bp: e.tensor_tensor(out=sgp[half][:], in0=sgp[half][:], in1=ps[:, bp, :], op=ALU.mult),
                       [], [r_sgp[half]], xreads=[r_ps[bp]])
                P.emit("pool", lambda e, half=half, b=b: e.tensor_tensor(
                    out=acc[:, b, half * 512:(half + 1) * 512], in0=acc[:, b, half * 512:(half + 1) * 512], in1=sgp[half][:], op=ALU.add),
                    [r_sgp[half]], [r_acc[b]])
            P.emit("dve", lambda e, b=b, f=f: e.scalar_tensor_tensor(
                out=acc[:, b, :], in0=xin[f][:], scalar=DN_ALPHA, in1=acc[:, b, :], op0=ALU.mult, op1=ALU.add),
                [r_xin[f]], [r_acc[b]])
            layer_norm_tok(P, acc[:, b, :], acc[:, b, :], g2, b2, r_acc[b], r_acc[b], r_c, lnt[f], lconst, r_c)
            P.dma("sp", out_x2[n * 128:(n + 1) * 128, :], acc[:, b, :], [r_acc[b]], [r_out], r_acc[b])


NQKV = 4352
A_FEAT_CHUNKS = list(range(0, 16)) + list(range(24, 33))


def phase_proj(P, nc, T, ps, r_ps, xT, qd, kd, qs, ks, vd, vs, r_out):
    xTb = P.sbuf("xTb", [128, 8, TOK], BF16)
    wb = P.sbuf("wb", [128, 8, NQKV], BF16)
    bcol = P.sbuf("bcol_t", [128, 50], F32)
    bq8 = P.sbuf("bq8", [128, 50], F32)
    bv = P.sbuf("bv", [128, 1152], F32)
    stage = [P.sbuf("stage%d" % i, [128, TOK], BF16) for i in range(2)]
    vstage = [P.sbuf("vstage%d" % i, [128, 1152], BF16) for i in range(2)]
    r_x = [P.res("x%d" % k, dma=True) for k in range(8)]
    r_w = [P.res("w%d" % k, dma=True) for k in range(8)]
    r_c = P.res("consts", dma=True)
    r_bq8 = P.res("bq8")
    r_stage = [P.res("stage%d" % i, dma=True) for i in range(2)]
    r_vstage = [P.res("vstage%d" % i, dma=True) for i in range(2)]

    xTv = xT.rearrange("(kc p) t -> p kc t", p=128)
    wv = T["w_in"].rearrange("(kc p) n -> p kc n", p=128)
    b_in = T["b_in"]
    P.dma("sp", bcol[:], T["bcol"], [], [r_c], r_c)
    P.dma("sp", bv[:, 0:1024], b_in[0:1, 2048:3072].to_broadcast([128, 1024]), [], [r_c], r_c)
    P.dma("sp", bv[:, 1024:1152], b_in[0:1, 4224:4352].to_broadcast([128, 128]), [], [r_c], r_c)
    for k in range(8):
        P.dma("pool", wb[:, k, :], wv[:, k, 0:NQKV], [], [r_w[k]], r_w[k])
        P.dma("pool", xTb[:, k, :], xTv[:, k, :], [], [r_x[k]], r_x[k])
    P.emit("dve", lambda h: h.tensor_scalar(out=bq8[:], in0=bcol[:], scalar1=0.125, scalar2=None, op0=ALU.mult),
           [r_c], [r_bq8])
    it = 0
    for mi, m in enumerate(A_FEAT_CHUNKS):
        st = mi % 2
        is_q = (m < 8) or (24 <= m < 32)
        for tg in range(8):
            bank = it % 4
            it += 1
            for k in range(8):
                P.emit("pe", lambda h, k=k, m=m, tg=tg, bank=bank: h.matmul(
                    ps[:, bank, :], lhsT=wb[:, k, m * 128:(m + 1) * 128], rhs=xTb[:, k, tg * 512:(tg + 1) * 512],
                    start=(k == 0), stop=(k == 7)), [r_w[k], r_x[k]], [r_ps[bank]])
            o = stage[st][:, tg * 512:(tg + 1) * 512]
            if it % 2 == 0:
                if is_q:
                    P.emit("act", lambda h, o=o, bank=bank, m=m: h.activation(
                        out=o, in_=ps[:, bank, :], func=AF.Identity, bias=bq8[:, m:m + 1], scale=0.125),
                        [r_bq8], [r_stage[st]], xreads=[r_ps[bank]])
                else:
                    P.emit("act", lambda h, o=o, bank=bank, m=m: h.activation(
                        out=o, in_=ps[:, bank, :], func=AF.Identity, bias=bcol[:, m:m + 1], scale=1.0),
                        [r_c], [r_stage[st]], xreads=[r_ps[bank]])
            else:
                P.emit("dve", lambda h, o=o, bank=bank, m=m, sc=(0.125 if is_q else 1.0): h.tensor_scalar(
                    out=o, in0=ps[:, bank, :], scalar1=bcol[:, m:m + 1], scalar2=sc, op0=ALU.add, op1=ALU.mult),
                    [r_c], [r_stage[st]], xreads=[r_ps[bank]])
        if m < 8:
            dst = qd[m]
        elif m < 16:
            dst = kd[m - 8]
        elif m < 32:
            dst = qs[m - 24]
        else:
            dst = ks
        P.dma("sp", dst, stage[st][:], [r_stage[st]], [r_out], r_stage[st])
    for n in range(NBLK):
        st = n % 2
        banks = [4, 5, 6]
        for j, (c0, w) in enumerate(((2048, 512), (2560, 512), (4224, 128))):
            for k in range(8):
                P.emit("pe", lambda h, k=k, n=n, c0=c0, w=w, bank=banks[j]: h.matmul(
                    ps[:, bank, 0:w], lhsT=xTb[:, k, n * 128:(n + 1) * 128], rhs=wb[:, k, c0:c0 + w],
                    start=(k == 0), stop=(k == 7)), [r_w[k], r_x[k]], [r_ps[banks[j]]])
        for j, (o0, w) in enumerate(((0, 512), (512, 512), (1024, 128))):
            P.emit("dve", lambda h, st=st, o0=o0, w=w, bank=banks[j]: h.tensor_tensor(
                out=vstage[st][:, o0:o0 + w], in0=ps[:, bank, 0:w], in1=bv[:, o0:o0 + w], op=ALU.add),
                [r_c], [r_vstage[st]], xreads=[r_ps[banks[j]]])
        P.dma("sp", vd[n * 128:(n + 1) * 128, :], vstage[st][:, 0:1024], [r_vstage[st]], [r_out], r_vstage[st])
        P.dma("sp", vs[n * 128:(n + 1) * 128, :], vstage[st][:, 1024:1152], [r_vstage[st]], [r_out], r_vstage[st])


def phase_transpose(P, nc, T, ps, psT, r_ps, r_psT, xsrc, xTdst, r_out):
    ident = P.sbuf("identb_t", [128, 128], BF16)
    xin = [P.sbuf("txin%d" % s, [128, 1024], F32) for s in range(2)]
    xb = [P.sbuf("txb%d" % s, [128, 1024], BF16) for s in range(2)]
    xT = [P.sbuf("txT%d" % s, [128, 8, 128], BF16) for s in range(2)]
    r_c = P.res("tc", dma=True)
    r_xin = [P.res("txin%d" % s, dma=True) for s in range(2)]
    r_xb = [P.res("txb%d" % s) for s in range(2)]
    r_xT = [P.res("txT%d" % s, dma=True) for s in range(2)]
    P.dma("pool", ident[:], T["ident"], [], [r_c], r_c)
    dstv = xTdst.rearrange("(kc p) t -> p kc t", p=128)
    for n in range(NBLK):
        s = n % 2
        P.dma("sp", xin[s][:], xsrc[n * 128:(n + 1) * 128, :], [], [r_xin[s]], r_xin[s])
        P.emit("pool", lambda e, s=s: e.tensor_copy(out=xb[s][:], in_=xin[s][:]), [r_xin[s]], [r_xb[s]])
        for k in range(8):
            P.emit("pe", lambda e, k=k, s=s: e.transpose(out=psT[:, k * 128:(k + 1) * 128], in_=xb[s][:, k * 128:(k + 1) * 128],
                                                         identity=ident[:]), [r_xb[s], r_c], [r_psT])
        P.emit("dve", lambda e, s=s: e.tensor_copy(out=xT[s][:].rearrange("p k t -> p (k t)"), in_=psT[:, :]),
               [], [r_xT[s]], xreads=[r_psT])
        P.dma("sp", dstv[:, :, n * 128:(n + 1) * 128], xT[s][:], [r_xT[s]], [r_out], r_xT[s])


def phase_select(P, nc, T, sel_ap, jobs, r_out):
    sel = P.sbuf("sel_t", [128, 4], F32)
    r_c = P.res("selc", dma=True)
    P.dma("sp", sel[:], sel_ap, [], [r_c], r_c)
    W = 2048
    cb = [[P.sbuf("selc%d_%d" % (s, i), [128, W], F32) for i in range(4)] for s in range(2)]
    ob = [P.sbuf("selo%d" % s, [128, W], F32) for s in range(2)]
    r_cb = [[P.res("selc%d_%d" % (s, i), dma=True) for i in range(4)] for s in range(2)]
    r_ob = [P.res("selo%d" % s, dma=True) for s in range(2)]
    it = 0
    for cands, out, dt in jobs:
        R, C = out.shape
        for r0 in range(0, R, 128):
            for c0 in range(0, C, W):
                w = min(W, C - c0)
                s = it % 2
                it += 1
                if dt is BF16:
                    cv = [cb[s][i][:].bitcast(BF16)[:, 0:w] for i in range(4)]
                    ov = ob[s][:].bitcast(BF16)[:, 0:w]
                else:
                    cv = [cb[s][i][:, 0:w] for i in range(4)]
                    ov = ob[s][:, 0:w]
                for i in range(4):
                    P.dma("sp", cv[i], cands[i][r0:r0 + 128, c0:c0 + w], [], [r_cb[s][i]], r_cb[s][i])
                P.emit("dve", lambda e, cv=cv, ov=ov: e.tensor_scalar(out=ov, in0=cv[0], scalar1=sel[:, 0:1], scalar2=None, op0=ALU.mult),
                       [r_cb[s][0], r_c], [r_ob[s]])
                for i in range(1, 4):
                    P.emit("dve", lambda e, cv=cv, ov=ov, i=i: e.scalar_tensor_tensor(
                        out=ov, in0=cv[i], scalar=sel[:, i:i + 1], in1=ov, op0=ALU.mult, op1=ALU.add),
                        [r_cb[s][i], r_c], [r_ob[s]])
                P.dma("sp", out[r0:r0 + 128, c0:c0 + w], ov, [r_ob[s]], [r_out], r_ob[s])


def phase_swa_sel(P, nc, T, sel_ap, ks_sm, vs_sm, ks2_cur, ks2_prev, vs_cur, vs_prev, r_out):
    sel = P.sbuf("ssel_t", [128, 4], F32)
    kall = P.sbuf("kall", [128, 4, TOK + 128], BF16)
    vall = P.sbuf("vall", [128, 4, NBLK + 1, 128], BF16)
    kc = P.sbuf("kcur", [128, TOK], BF16)
    kp = P.sbuf("kprev", [128, TOK], BF16)
    vc = P.sbuf("vcur", [128, NBLK, 128], BF16)
    vp = P.sbuf("vprev", [128, NBLK, 128], BF16)
    r_c = P.res("sselc", dma=True)
    r_k = P.res("kall", dma=True)
    r_v = P.res("vall", dma=True)
    r_o = [P.res("ssel_o%d" % i, dma=True) for i in range(4)]
    P.dma("sp", sel[:], sel_ap, [], [r_c], r_c)
    P.emit("dve", lambda e: e.memset(kall[:, :, 0:128], 0.0), [], [r_k])
    P.emit("dve", lambda e: e.memset(vall[:, :, 0, :], 0.0), [], [r_v])
    vsv = vs_sm.rearrange("(s n p) c -> s p n c", s=4, p=128)
    for i in range(4):
        P.dma("sp", kall[:, i, 128:], ks_sm[i], [], [r_k], r_k)
        P.dma("sp", vall[:, i, 1:, :], vsv[i], [], [r_v], r_v)
    def kview(i, shift):
        return kall[:, i, 128 - shift * 128:128 - shift * 128 + TOK]

    def vview(i, shift):
        return vall[:, i, 1 - shift:1 - shift + NBLK, :]

    for (dst, view, rr, cand) in ((kc, kview, r_k, [(0, 0), (1, 0), (2, 0), (3, 0)]), (kp, kview, r_k, [(3, 1), (0, 0), (1, 0), (2, 0)]),
                                  (vc, vview, r_v, [(0, 0), (1, 0), (2, 0), (3, 0)]), (vp, vview, r_v, [(3, 1), (0, 0), (1, 0), (2, 0)])):
        ro = r_o[[kc, kp, vc, vp].index(dst)]
        P.emit("dve", lambda e, dst=dst, view=view, cand=cand: e.tensor_scalar(
            out=dst[:], in0=view(*cand[0]), scalar1=sel[:, 0:1], scalar2=None, op0=ALU.mult), [rr, r_c], [ro])
        for i in range(1, 4):
            P.emit("dve", lambda e, dst=dst, view=view, cand=cand, i=i: e.scalar_tensor_tensor(
                out=dst[:], in0=view(*cand[i]), scalar=sel[:, i:i + 1], in1=dst[:], op0=ALU.mult, op1=ALU.add), [rr, r_c], [ro])
    for (src, dstd, ro) in ((kc, ks2_cur, r_o[0]), (kp, ks2_prev, r_o[1])):
        for g in range(2):
            for dup in range(2):
                P.dma("sp", dstd[g][dup * 64:(dup + 1) * 64, :], src[g * 64:(g + 1) * 64, :], [ro], [r_out], ro)
    P.dma("sp", vs_cur.rearrange("(n p) c -> p n c", p=128), vc[:], [r_o[2]], [r_out], r_o[2])
    P.dma("sp", vs_prev.rearrange("(n p) c -> p n c", p=128), vp[:], [r_o[3]], [r_out], r_o[3])


def build_A():
    nc = bass.Bass("TRN2", target_bir_lowering=False)
    T = {}
    xT = nc.dram_tensor("xT", [D, TOK], F32, kind="ExternalInput").ap()
    T["w_in"] = nc.dram_tensor("w_in", [D, 6400], F32, kind="ExternalInput").ap()
    T["bcol"] = nc.dram_tensor("bcol", [128, 50], F32, kind="ExternalInput").ap()
    T["b_in"] = nc.dram_tensor("b_in", [1, 6400], F32, kind="ExternalInput").ap()
    qd = nc.dram_tensor("qd", [8, 128, TOK], BF16, kind="ExternalOutput").ap()
    kd = nc.dram_tensor("kd", [8, 128, TOK], BF16, kind="ExternalOutput").ap()
    qs = nc.dram_tensor("qs", [8, 128, TOK], BF16, kind="ExternalOutput").ap()
    ks = nc.dram_tensor("ks", [128, TOK], BF16, kind="ExternalOutput").ap()
    vd = nc.dram_tensor("vd", [TOK, 1024], BF16, kind="ExternalOutput").ap()
    vs = nc.dram_tensor("vs", [TOK, 128], BF16, kind="ExternalOutput").ap()
    with ExitStack() as stack:
        P = Prog(nc, stack)
        ps = P.psum("ps", [128, 7, 512], F32)
        r_ps = [P.res("psb%d" % i) for i in range(7)]
        r_out = P.res("out")
        phase_proj(P, nc, T, ps, r_ps, xT, qd, kd, qs, ks, vd, vs, r_out)
        P.finish()
    return nc


def build_B():
    nc = bass.Bass("TRN2", target_bir_lowering=False)
    T = {}

    def din(name, shape, dt=F32):
        T[name] = nc.dram_tensor(name, list(shape), dt, kind="ExternalInput").ap()

    din("qd", [8, 128, TOK], BF16)
    din("kd_all", [8, 128, SEQ], BF16)
    din("vd_all", [SEQ, 1024], BF16)
    din("abias", [128, 1024])
    din("mask4", [128, 4 * 2 * GQ * 128])
    din("ident", [128, 128])
    din("lconst", [128, 4])
    for nm in ("lambda_q1", "lambda_k1", "lambda_q2", "lambda_k2"):
        din(nm, [1, 64])
    din("subln_w", [1, 128])
    din("qs", [8, 128, TOK], BF16)
    din("ks2_prev", [2, 128, TOK], BF16)
    din("ks2_cur", [2, 128, TOK], BF16)
    din("vs_prev", [TOK, 128], BF16)
    din("vs_cur", [TOK, 128], BF16)
    din("sbias", [128, 32])
    din("smask", [128, 3 * 1024])
    din("sinkc", [128, 16])
    din("sinks", [1, 16])
    din("w_in", [D, 6400])
    din("bcol", [128, 50])
    din("w_br_diff", [D, D])
    din("w_br_swa", [D, D])
    din("w_out", [D, D])
    din("b_out", [1, D])
    din("ln1_g", [1, D])
    din("ln1_b", [1, D])
    din("xT", [D, TOK])
    din("x", [TOK, D])
    din("w_router", [D, E])
    din("b_router", [1, E])
    din("bgu_col", [128, E * 16])
    din("b_down", [E, D])
    din("w_gate_up", [E, D, 2 * D])
    din("w_down", [E, D, D])
    din("w_ple_gate", [D, D])
    din("w_ple_proj", [256, D])
    din("pT", [256, TOK])
    din("ln2_g", [1, D])
    din("ln2_b", [1, D])
    x2 = nc.dram_tensor("x2", [TOK, D], F32, kind="ExternalOutput").ap()
    odT = nc.dram_tensor("odT", [1024, TOK], BF16, kind="Internal").ap()
    osT = nc.dram_tensor("osT", [1024, TOK], BF16, kind="Internal").ap()
    x1s = nc.dram_tensor("x1s", [TOK, D], F32, kind="Internal").ap()
    x1T = nc.dram_tensor("x1T", [D, TOK], BF16, kind="Internal").ap()
    with ExitStack() as stack:
        P = Prog(nc, stack)
        ps = P.psum("ps", [128, 7, 512], F32)
        psT = P.psum("psT", [128, 1024], BF16)
        r_ps = [P.res("psb%d" % i) for i in range(7)]
        r_psT = P.res("psT")
        r_scr = P.res("scratch")
        r_out = P.res("out")
        for fn, args in ((phase_diff_attn, (odT, r_scr)), (phase_swa, (osT, r_scr)),
                         (phase_post, (odT, osT, x1s, x1T, r_scr)), (phase_moe, (x1s, x1T, x2, r_out))):
            with ExitStack() as st2:
                P.stack = st2
                P.phase_begin()
                fn(P, nc, T, ps, psT, r_ps, r_psT, *args)
                P.barrier()
        P.stack = stack
        P.finish()
    return nc


_NC_CACHE = {}


def _get(name, fn):
    if name not in _NC_CACHE:
        _NC_CACHE[name] = fn()
    return _NC_CACHE[name]


def _own_rows(arr_b, r):
    return arr_b.reshape(NBLK, 4, 128, -1)[:, r].reshape(TOK, -1)


def kernel(**inp):
    f32 = np.float32
    ncA = _get("A", build_A)
    ncB = _get("B", build_B)
    cores = list(range(NCORES))
    xcur = [np.ascontiguousarray(_own_rows(np.asarray(inp["x"][c // 4], f32), c % 4)) for c in cores]
    for L in range(DEPTH):
        w_in = np.ascontiguousarray(inp["w_in"][L], f32)
        b_in = np.asarray(inp["b_in"][L], f32)
        bcol = np.ascontiguousarray(b_in.reshape(50, 128).T)
        xT = [np.ascontiguousarray(xc.T) for xc in xcur]
        mapsA = [{"xT": xT[c], "w_in": w_in, "bcol": bcol, "b_in": b_in[None, :]} for c in cores]
        resA = run_bass_kernel_spmd(ncA, mapsA, core_ids=cores).results
        kd_all, vd_all, ks_all, vs_all = [], [], [], []
        for b in range(BATCH):
            rs = [resA[4 * b + r] for r in range(4)]
            kd_all.append(np.ascontiguousarray(
                np.stack([x_["kd"].reshape(8, 128, NBLK, 128) for x_ in rs], axis=3).reshape(8, 128, SEQ)))
            vd_all.append(np.ascontiguousarray(
                np.stack([x_["vd"].reshape(NBLK, 128, 1024) for x_ in rs], axis=1).reshape(SEQ, 1024)))
            ks_all.append(np.stack([x_["ks"].reshape(128, NBLK, 128) for x_ in rs], axis=2).reshape(128, NCH, 128))
            vs_all.append(np.stack([x_["vs"].reshape(NBLK, 128, 128) for x_ in rs], axis=1).reshape(NCH, 128, 128))
        mapsB = []
        for c in cores:
            b, r = c // 4, c % 4
            blk_cur = np.arange(NBLK) * 4 + r
            blk_prev = blk_cur - 1
            ksb = ks_all[b]
            ks_cur = ksb[:, blk_cur, :].reshape(2, 64, TOK)
            ks_prev = ksb[:, np.maximum(blk_prev, 0), :].copy()
            vs_prev = vs_all[b][np.maximum(blk_prev, 0)].copy()
            if blk_prev[0] < 0:
                ks_prev[:, 0, :] = 0
                vs_prev[0] = 0
            ks_prev = ks_prev.reshape(2, 64, TOK)
            m = {
                "qd": resA[c]["qd"], "kd_all": kd_all[b], "vd_all": vd_all[b], "qs": resA[c]["qs"],
                "ks2_cur": np.ascontiguousarray(np.concatenate([ks_cur, ks_cur], axis=1)),
                "ks2_prev": np.ascontiguousarray(np.concatenate([ks_prev, ks_prev], axis=1)),
                "vs_cur": np.ascontiguousarray(vs_all[b][blk_cur].reshape(TOK, 128)),
                "vs_prev": np.ascontiguousarray(vs_prev.reshape(TOK, 128)),
                "w_in": w_in, "bcol": bcol, "xT": xT[c], "x": xcur[c],
                "pT": np.ascontiguousarray(_own_rows(np.asarray(inp["p"][L][b], f32), r).T),
                "bgu_col": np.ascontiguousarray(
                    np.asarray(inp["b_gate_up"][L], f32).reshape(E, 16, 128).transpose(2, 0, 1).reshape(128, E * 16)),
            }
            m.update(host_consts(r, L))
            m.update(host_consts_swa(r))
            for nm in ("lambda_q1", "lambda_k1", "lambda_q2", "lambda_k2", "subln_w", "sinks", "b_out", "ln1_g", "ln1_b",
                       "b_router", "ln2_g", "ln2_b"):
                m[nm] = np.ascontiguousarray(np.asarray(inp[nm][L], f32)[None, :])
            for nm in ("w_br_diff", "w_br_swa", "w_out", "w_router", "b_down", "w_gate_up", "w_down", "w_ple_gate", "w_ple_proj"):
                m[nm] = np.ascontiguousarray(inp[nm][L], f32)
            mapsB.append(m)
        resB = run_bass_kernel_spmd(ncB, mapsB, core_ids=cores).results
        xcur = [resB[c]["x2"] for c in cores]
    out = np.zeros((BATCH, SEQ, D), f32)
    for c in cores:
        b, r = c // 4, c % 4
        out[b].reshape(NBLK, 4, 128, D)[:, r] = xcur[c].reshape(NBLK, 128, D)
    return out
```
